# Optimizing a Trainium2 kernel written in Bass

```python
import math
import jax, jax.numpy as jnp
from jax import lax
import numpy as np

D_MODEL = 1024
BATCH = 8
SEQ = 4096
DEPTH = 4

GLA_HEADS = 4
GLA_DK = 64
GLA_DV = 128
GLA_RANK = 16
GLA_TAU = 16.0
GLA_CHUNK = 64
DIFF_HEADS = 4
DIFF_DH = 64
DIFF_DV = 2 * DIFF_DH
Q_BLOCK = 128
A_QK = GLA_HEADS * GLA_DK
A_V = GLA_HEADS * GLA_DV
B_QK = DIFF_HEADS * 2 * DIFF_DH
B_V = DIFF_HEADS * DIFF_DV
EVEN_IN = 2 * A_QK + 2 * A_V + GLA_RANK + 2 * B_QK + B_V
EVEN_MIX = A_V + B_V
DIL_PATTERNS = ((128, 1), (512, 4), (2048, 16))
DIL_GROUPS = 3
DIL_HEADS = 4
DIL_DH = 128
ODD_IN = DIL_GROUPS * 3 * DIL_HEADS * DIL_DH
ODD_MIX = DIL_HEADS * DIL_DH
D_FF = 2816
CONV_W = 3
EPS = 1e-6
N_EVEN = (DEPTH + 1) // 2
N_ODD = DEPTH // 2

kernel_name = "hybrid_gla_diffattn_dilated_convffn"


def rmsnorm(x, g):
    xf = x.astype(jnp.float32)
    y = xf * lax.rsqrt(jnp.mean(xf * xf, axis=-1, keepdims=True) + EPS)
    return (y * g.astype(jnp.float32)).astype(x.dtype)


def gla(q, k, v, log_a):
    B, S, H, dk = q.shape
    dv = v.shape[-1]
    C = GLA_CHUNK
    N = S // C
    f32 = jnp.float32

    def chunk(t):
        return t.astype(f32).reshape(B, N, C, H, t.shape[-1]).transpose(0, 3, 1, 2, 4)

    qc, kc, vc, lc = chunk(q * (dk ** -0.5)), chunk(k), chunk(v), chunk(log_a)
    b = jnp.cumsum(lc, axis=3)
    b_last = b[:, :, :, -1:, :]
    q_d = qc * jnp.exp(b)
    k_d = kc * jnp.exp(-b)
    causal = jnp.tril(jnp.ones((C, C), dtype=bool))
    att = jnp.where(causal, jnp.einsum('bhnik,bhnjk->bhnij', q_d, k_d), 0.0)
    o_intra = jnp.einsum('bhnij,bhnjv->bhniv', att, vc)
    kv = jnp.einsum('bhnjk,bhnjv->bhnkv', kc * jnp.exp(b_last - b), vc)
    decay = jnp.exp(b_last[:, :, :, 0, :])

    def step(state, inp):
        kv_n, dec_n = inp
        return dec_n[..., None] * state + kv_n, state

    s0 = jnp.zeros((B, H, dk, dv), f32)
    _, states = lax.scan(step, s0, (jnp.moveaxis(kv, 2, 0), jnp.moveaxis(decay, 2, 0)))
    states = jnp.moveaxis(states, 0, 2)
    o = o_intra + jnp.einsum('bhnik,bhnkv->bhniv', q_d, states)
    return o.transpose(0, 2, 3, 1, 4).reshape(B, S, H, dv).astype(v.dtype)


def diff_attention(q, k, v, lam):
    B, S, H, _, dh = q.shape
    dv = v.shape[-1]
    nb = S // Q_BLOCK
    qb = (q * (dh ** -0.5)).reshape(B, nb, Q_BLOCK, H, 2, dh).transpose(1, 0, 3, 4, 2, 5)
    kt = k.transpose(0, 2, 3, 1, 4)
    vt = v.transpose(0, 2, 1, 3)
    kpos = jnp.arange(S)

    def block(args):
        qblk, n = args
        s = jnp.einsum('bhcqd,bhckd->bhcqk', qblk, kt).astype(jnp.float32)
        qpos = n * Q_BLOCK + jnp.arange(Q_BLOCK)
        s = jnp.where(kpos[None, :] <= qpos[:, None], s, -jnp.inf)
        p = jax.nn.softmax(s, axis=-1)
        w = p[:, :, 0] - lam * p[:, :, 1]
        return jnp.einsum('bhqk,bhkv->bhqv', w.astype(vt.dtype), vt)

    o = lax.map(block, (qb, jnp.arange(nb)))
    return o.transpose(1, 0, 3, 2, 4).reshape(B, S, H, dv)


def dilated_branch(q, k, v, dilation, wc):
    B, S, H, dh = q.shape
    L = S // dilation
    nb = -(-L // wc)
    Lp = nb * wc

    def to_class(t):
        t = t.reshape(B, L, dilation, H, dh).transpose(0, 2, 3, 1, 4)
        t = jnp.pad(t, ((0, 0), (0, 0), (0, 0), (0, Lp - L), (0, 0)))
        return t.reshape(B, dilation, H, nb, wc, dh)

    def band(t):
        prev = jnp.pad(t, ((0, 0),) * 3 + ((1, 0), (0, 0), (0, 0)))[:, :, :, :-1]
        return jnp.concatenate([prev, t], axis=-2)

    qc = to_class(q)
    kb = band(to_class(k))
    vb = band(to_class(v))
    s = jnp.einsum('brhnqd,brhnkd->brhnqk', qc, kb).astype(jnp.float32) * (dh ** -0.5)
    i = jnp.arange(wc)[:, None]
    j = jnp.arange(2 * wc)[None, :]
    dist = i + wc - j
    blk = jnp.arange(nb)[:, None, None]
    valid = (dist >= 0) & (dist <= wc) & ((blk > 0) | (j >= wc))
    s = jnp.where(valid, s, -jnp.inf)
    lse = jax.nn.logsumexp(s, axis=-1)
    p = jnp.exp(s - lse[..., None])
    o = jnp.einsum('brhnqk,brhnkd->brhnqd', p.astype(vb.dtype), vb)
    o = o.reshape(B, dilation, H, Lp, dh)[:, :, :, :L].transpose(0, 3, 1, 2, 4).reshape(B, S, H, dh)
    lse = lse.reshape(B, dilation, H, Lp)[..., :L].transpose(0, 3, 1, 2).reshape(B, S, H)
    return o, lse


def even_mixer(h, w_in, w_a2, b_a, gla_gain, dq_gain, dk_gain, lq1, lk1, lq2, lk2, diff_gain, w_out, lambda_init):
    B, S, _ = h.shape
    z = h @ w_in
    sizes = [A_QK, A_QK, A_V, A_V, GLA_RANK, B_QK, B_QK]
    aq, ak, av, ag, ar, bq, bk, bv = jnp.split(z, [int(c) for c in np.cumsum(sizes)], axis=-1)
    log_a = jax.nn.log_sigmoid((ar @ w_a2 + b_a).astype(jnp.float32)) / GLA_TAU
    oa = gla(aq.reshape(B, S, GLA_HEADS, GLA_DK), ak.reshape(B, S, GLA_HEADS, GLA_DK),
             av.reshape(B, S, GLA_HEADS, GLA_DV), log_a.reshape(B, S, GLA_HEADS, GLA_DK))
    oa = rmsnorm(oa, gla_gain) * jax.nn.silu(ag.reshape(B, S, GLA_HEADS, GLA_DV))
    bq = rmsnorm(bq.reshape(B, S, DIFF_HEADS, 2, DIFF_DH), dq_gain)
    bk = rmsnorm(bk.reshape(B, S, DIFF_HEADS, 2, DIFF_DH), dk_gain)
    f32 = jnp.float32
    lam = (jnp.exp(jnp.sum(lq1.astype(f32) * lk1.astype(f32)))
           - jnp.exp(jnp.sum(lq2.astype(f32) * lk2.astype(f32))) + lambda_init)
    ob = diff_attention(bq, bk, bv.reshape(B, S, DIFF_HEADS, DIFF_DV), lam)
    ob = rmsnorm(ob, diff_gain) * (1.0 - lambda_init)
    o = jnp.concatenate([oa.reshape(B, S, A_V), ob.reshape(B, S, B_V)], axis=-1)
    return o @ w_out


def odd_mixer(h, w_in, q_gain, k_gain, w_out):
    B, S, _ = h.shape
    z = (h @ w_in).reshape(B, S, DIL_GROUPS, 3, DIL_HEADS, DIL_DH)
    q = rmsnorm(z[:, :, :, 0], q_gain)
    k = rmsnorm(z[:, :, :, 1], k_gain)
    v = z[:, :, :, 2]
    outs, lses = [], []
    for g, (window, dilation) in enumerate(DIL_PATTERNS):
        o_g, lse_g = dilated_branch(q[:, :, g], k[:, :, g], v[:, :, g], dilation, window // dilation)
        outs.append(o_g)
        lses.append(lse_g)
    wts = jax.nn.softmax(jnp.stack(lses, axis=0), axis=0)
    o = jnp.sum(wts[..., None].astype(v.dtype) * jnp.stack(outs, axis=0), axis=0)
    return o.reshape(B, S, ODD_MIX) @ w_out


def conv_ffn(h, w_up, conv_w, conv_b, w_down):
    u = h @ w_up
    u = lax.conv_general_dilated(u, conv_w, window_strides=(1,), padding=[(CONV_W - 1, 0)],
                                 dimension_numbers=('NWC', 'WIO', 'NWC'),
                                 feature_group_count=2 * D_FF) + conv_b
    gate, val = jnp.split(u, 2, axis=-1)
    return (jax.nn.silu(gate) * val) @ w_down


def setup_inputs(seed: int = 0) -> dict:
    key = jax.random.key(seed)
    ks = iter(jax.random.split(key, 32))
    nrm = lambda shape, scale: jax.random.normal(next(ks), shape, jnp.float32) * scale
    gain = lambda shape: 1.0 + nrm(shape, 0.02)
    return {
        "x": nrm((BATCH, SEQ, D_MODEL), 1.0),
        "norm_mix": gain((DEPTH, D_MODEL)),
        "norm_ffn": gain((DEPTH, D_MODEL)),
        "ev_w_in": nrm((N_EVEN, D_MODEL, EVEN_IN), D_MODEL ** -0.5),
        "ev_w_a2": nrm((N_EVEN, GLA_RANK, A_QK), GLA_RANK ** -0.5),
        "ev_b_a": nrm((N_EVEN, A_QK), 0.1),
        "ev_gla_gain": gain((N_EVEN, GLA_DV)),
        "ev_dq_gain": gain((N_EVEN, DIFF_DH)),
        "ev_dk_gain": gain((N_EVEN, DIFF_DH)),
        "ev_lq1": nrm((N_EVEN, DIFF_DH), 0.1),
        "ev_lk1": nrm((N_EVEN, DIFF_DH), 0.1),
        "ev_lq2": nrm((N_EVEN, DIFF_DH), 0.1),
        "ev_lk2": nrm((N_EVEN, DIFF_DH), 0.1),
        "ev_diff_gain": gain((N_EVEN, DIFF_DV)),
        "ev_w_out": nrm((N_EVEN, EVEN_MIX, D_MODEL), EVEN_MIX ** -0.5),
        "od_w_in": nrm((N_ODD, D_MODEL, ODD_IN), D_MODEL ** -0.5),
        "od_q_gain": gain((N_ODD, DIL_DH)),
        "od_k_gain": gain((N_ODD, DIL_DH)),
        "od_w_out": nrm((N_ODD, ODD_MIX, D_MODEL), ODD_MIX ** -0.5),
        "ffn_w_up": nrm((DEPTH, D_MODEL, 2 * D_FF), D_MODEL ** -0.5),
        "ffn_conv_w": nrm((DEPTH, CONV_W, 1, 2 * D_FF), CONV_W ** -0.5),
        "ffn_conv_b": nrm((DEPTH, 2 * D_FF), 0.01),
        "ffn_w_down": nrm((DEPTH, D_FF, D_MODEL), D_FF ** -0.5),
    }


def reference(x, norm_mix, norm_ffn, ev_w_in, ev_w_a2, ev_b_a, ev_gla_gain, ev_dq_gain, ev_dk_gain,
              ev_lq1, ev_lk1, ev_lq2, ev_lk2, ev_diff_gain, ev_w_out, od_w_in, od_q_gain, od_k_gain,
              od_w_out, ffn_w_up, ffn_conv_w, ffn_conv_b, ffn_w_down):
    for i in range(DEPTH):
        h = rmsnorm(x, norm_mix[i])
        if i % 2 == 0:
            e = i // 2
            lambda_init = 0.8 - 0.6 * math.exp(-0.3 * i)
            x = x + even_mixer(h, ev_w_in[e], ev_w_a2[e], ev_b_a[e], ev_gla_gain[e], ev_dq_gain[e],
                               ev_dk_gain[e], ev_lq1[e], ev_lk1[e], ev_lq2[e], ev_lk2[e],
                               ev_diff_gain[e], ev_w_out[e], lambda_init)
        else:
            o = i // 2
            x = x + odd_mixer(h, od_w_in[o], od_q_gain[o], od_k_gain[o], od_w_out[o])
        x = x + conv_ffn(rmsnorm(x, norm_ffn[i]), ffn_w_up[i], ffn_conv_w[i], ffn_conv_b[i], ffn_w_down[i])
    return x
```

```python
import numpy as np
from contextlib import ExitStack
import concourse.bass as bass
import concourse.mybir as mybir
from concourse.bass_utils import run_bass_kernel_spmd
F32 = mybir.dt.float32
BF16 = mybir.dt.bfloat16
AF = mybir.ActivationFunctionType
ALU = mybir.AluOpType
AX = mybir.AxisListType

SEM_LIMIT = 30000


class Tile:
    __slots__ = ("name", "ap", "w", "r")

    def __init__(self, name, ap):
        self.name = name
        self.ap = ap
        self.w = None
        self.r = []

    def __getitem__(self, k):
        return self.ap[k]


class Op:
    __slots__ = ("eng", "fn", "waits", "sig", "ms", "semkey", "dma", "idx")


class Sched:
    ENGS = ("pe", "act", "dve", "pool", "sp")

    def __init__(self, nc, es, sync_same_engine=True):
        self.nc = nc
        self.es = es
        self.ops = {e: [] for e in self.ENGS}
        self.nsig = {e: 0 for e in self.ENGS}
        self.dma_cnt = {}
        self.known = {e: {} for e in self.ENGS}
        self.sync_same = sync_same_engine
        self.sems = {}
        self.n_ops = 0
        self.pending = {e: [] for e in self.ENGS}

    def op(self, eng, fn, reads=(), writes=(), dma=None):
        o = Op()
        o.eng = eng
        o.fn = fn
        o.sig = False
        o.ms = None
        o.dma = dma
        o.idx = self.n_ops
        self.n_ops += 1
        if self.pending[eng]:
            reads = list(reads) + self.pending[eng]
            self.pending[eng] = []
        deps = []
        for t in reads:
            if t.w is not None:
                deps.append(t.w)
        for t in writes:
            if t.w is not None:
                deps.append(t.w)
            deps.extend(t.r)
        cdeps = {}
        dwaits = {}
        for p in deps:
            if p is o:
                continue
            if p.dma is not None:
                val = self.dma_cnt[p.dma] * 16
                if dwaits.get(p.dma, 0) < val:
                    dwaits[p.dma] = val
            else:
                if p.eng == eng and (eng == "pe" or not self.sync_same):
                    continue
                p.sig = True
                q = cdeps.get(p.eng)
                if q is None or q.idx < p.idx:
                    cdeps[p.eng] = p
        o.waits = (list(cdeps.values()), list(dwaits.items()))
        if dma is not None:
            self.dma_cnt[dma] = self.dma_cnt.get(dma, 0) + 1
        for t in reads:
            t.r.append(o)
        for t in writes:
            t.w = o
            t.r = []
        self.ops[eng].append(o)
        return o

    def _sem(self, key):
        s = self.sems.get(key)
        if s is None:
            s = self.es.enter_context(self.nc.semaphore("s_%s_%s" % (key[0], str(key[1]).replace(" ", "_"))))
            self.sems[key] = s
        return s

    def emit(self, final_waits=()):
        nc = self.nc
        block = self.es.enter_context(nc.Block())
        for e in self.ENGS:
            n = 0
            for o in self.ops[e]:
                if o.dma is None and o.sig:
                    n += 1
                    o.ms = n
        for e in self.ENGS:
            kn = {}
            for o in self.ops[e]:
                cd, dw = o.waits
                w = {}
                for p in cd:
                    key = (p.eng, (p.ms - 1) // SEM_LIMIT)
                    val = (p.ms - 1) % SEM_LIMIT + 1
                    if w.get(key, 0) < val:
                        w[key] = val
                for k, val in dw:
                    key = ("dma", k)
                    if w.get(key, 0) < val:
                        w[key] = val
                res = []
                for key, val in w.items():
                    if kn.get(key, 0) >= val:
                        continue
                    kn[key] = val
                    res.append((key, val))
                    self._sem(key)
                o.waits = res
                if o.dma is not None:
                    self._sem(("dma", o.dma))
                elif o.sig:
                    self._sem((o.eng, (o.ms - 1) // SEM_LIMIT))
        for key, cnt in final_waits:
            self._sem(("dma", key))

        def run(ename):
            def body(eng):
                for o in self.ops[ename]:
                    for key, val in o.waits:
                        eng.wait_ge(self.sems[key], val)
                    ins = o.fn(eng)
                    if o.dma is not None:
                        ins.then_inc(self.sems[("dma", o.dma)], 16)
                    elif o.sig:
                        ins.then_inc(self.sems[(o.eng, (o.ms - 1) // SEM_LIMIT)], 1)
                if ename == "sp":
                    for key, cnt in final_waits:
                        eng.wait_ge(self.sems[("dma", key)], cnt * 16)
            return body

        if self.ops["sp"] or final_waits:
            block.sync(run("sp"))
        if self.ops["pe"]:
            block.tensor(run("pe"))
        if self.ops["act"]:
            block.scalar(run("act"))
        if self.ops["dve"]:
            block.vector(run("dve"))
        if self.ops["pool"]:
            block.gpsimd(run("pool"))


    def barrier(self):
        ts = []
        for e in ("pe", "act", "dve", "pool"):
            if self.ops[e]:
                t = Tile("bar_" + e, None)
                t.w = self.ops[e][-1]
                ts.append(t)
        for key in list(self.dma_cnt.keys()):
            t = Tile("bard_" + key, None)
            o = Op()
            o.eng = "sp"; o.dma = key; o.sig = False; o.ms = None; o.idx = -1
            t.w = o
            ts.append(t)
        for e in self.ENGS:
            self.pending[e] = list(ts)


class Ring:
    def __init__(self, tiles):
        self.tiles = tiles
        self.i = 0

    def next(self):
        t = self.tiles[self.i % len(self.tiles)]
        self.i += 1
        return t


D = 1024
DFF = 2816
NJ = DFF // 128
EPS = 1e-6
TT = 512
USE_F32R = True
F32R = mybir.dt.float32r


def r32(ap):
    return ap.bitcast(F32R) if USE_F32R else ap


class KB:
    def __init__(self, nc, es, S):
        self.nc = nc
        self.es = es
        self.S = S
        self.NT = S // TT
        self.sc = Sched(nc, es)
        self.dram = {}
        self.psum = []
        for i in range(8):
            t = es.enter_context(nc.psum_tensor("psb%d" % i, [128, 512], F32))
            self.psum.append(t)
        self.ones32_t = es.enter_context(nc.sbuf_tensor("ones32", [128, 128], F32))
        self.ones32 = Tile("ones32", self.ones32_t[:])
        self.ones32raw_t = es.enter_context(nc.sbuf_tensor("ones32raw", [128, 128], F32))
        raw = Tile("ones32raw", self.ones32raw_t[:])
        self.sc.op("pool", lambda e: e.memset(self.ones32raw_t[:], 1.0), writes=[raw])
        self.sc.op("dve", lambda e: e.tensor_copy(r32(self.ones32_t[:]), self.ones32raw_t[:]), reads=[raw], writes=[self.ones32])

    def din(self, name, shape, dt=F32):
        t = self.nc.dram_tensor(name, list(shape), dt, kind="ExternalInput")
        self.dram[name] = t
        return t.ap()

    def dout(self, name, shape, dt=F32):
        t = self.nc.dram_tensor(name, list(shape), dt, kind="ExternalOutput")
        self.dram[name] = t
        return t.ap()

    def dscr(self, name, shape, dt):
        t = self.nc.dram_tensor(name, list(shape), dt, kind="Internal")
        self.dram[name] = t
        return t.ap()

    def sb(self, pes, name, shape, dt):
        self._uid = getattr(self, "_uid", 0) + 1
        return pes.enter_context(self.nc.sbuf_tensor("%s_u%d" % (name, self._uid), list(shape), dt))

    def ptiles(self, tag):
        return [Tile("%s_ps%d" % (tag, i), self.psum[i][:]) for i in range(8)]

    def barrier(self):
        self.sc.barrier()


def prep_ffn_weights(kb, L, w_up, w_down, wup_s, wdn_s):
    sc = kb.sc
    src_up = w_up[L].rearrange("(kc p) (gv j c) -> j p gv kc c", p=128, gv=2, j=NJ)
    src_dn = w_down[L].rearrange("(j p) (d c) -> d p j c", p=128, d=8)
    t_up = kb.wt[("up", L)]
    t_dn = kb.wt[("dn", L)]
    th = []
    for j in range(NJ):
        th.append(lambda j=j: sc.op("pool", lambda e: e.dma_start(out=wup_s[L, j], in_=src_up[j]),
                                    writes=[t_up[j]], dma="wprep%d" % L))
    for d in range(8):
        th.append(lambda d=d: sc.op("pool", lambda e: e.dma_start(out=wdn_s[L, d], in_=src_dn[d]),
                                    writes=[t_dn[d]], dma="wprep%d" % L))
    return th


def phase_ffn(kb, L, x_src, xs_t, x_dst, xd_t, wup_s, wdn_s, vec, mix=None, hook=None):
    nc, sc, S, NT = kb.nc, kb.sc, kb.S, kb.NT
    with ExitStack() as pes:
        xT = kb.sb(pes, "f_x", [128, 2, 8, TT], F32)
        hT = kb.sb(pes, "f_h", [128, 2, 8, TT], BF16)
        sqT = kb.sb(pes, "f_sq", [128, 8, TT], F32)
        lnT = kb.sb(pes, "f_ln", [128, TT], F32)
        rsT = kb.sb(pes, "f_rs", [128, TT], F32)
        aT = kb.sb(pes, "f_a", [128, NJ, TT], BF16)
        gT = kb.sb(pes, "f_g", [128, 3, TT], F32)
        vT = kb.sb(pes, "f_v", [128, 3, TT], F32)
        wuT = kb.sb(pes, "f_wu", [128, 6, 2, 8, 128], BF16)
        wdT = kb.sb(pes, "f_wd", [128, 4, NJ, 128], BF16)
        hlT = kb.sb(pes, "f_hl", [128, 2, 2 * NJ, 2], F32)
        x_r = Ring([Tile("x%d" % i, xT[:, i]) for i in range(2)])
        h_r = Ring([Tile("h%d" % i, hT[:, i]) for i in range(2)])
        sq_r = Ring([Tile("sq%d" % i, sqT[:, i]) for i in range(8)])
        ln_t = Tile("ln", lnT[:])
        rs_t = Tile("rs", rsT[:])
        a_t = [Tile("a%d" % j, aT[:, j]) for j in range(NJ)]
        g_r = Ring([Tile("g%d" % i, gT[:, i]) for i in range(3)])
        v_r = Ring([Tile("v%d" % i, vT[:, i]) for i in range(3)])
        wu_r = Ring([Tile("wu%d" % i, wuT[:, i]) for i in range(6)])
        wd_r = Ring([Tile("wd%d" % i, wdT[:, i]) for i in range(4)])
        hl_t = [[Tile("hl%d_%d" % (pp_, q), hlT[:, pp_, q]) for q in range(2 * NJ)] for pp_ in range(2)]
        ps = kb.ptiles("f")
        ss_ps = ps[0]
        gp_r = Ring([ps[1], ps[2]])
        vp_r = Ring([ps[3], ps[4]])
        y_r = Ring([ps[5], ps[6], ps[7]])
        if mix is not None:
            oT_d, wout_s, KC = mix
            oT = kb.sb(pes, "f_o", [128, 2, KC, TT], BF16)
            woT = kb.sb(pes, "f_wo", [128, KC, D], BF16)
            o_r = Ring([Tile("o%d" % i, oT[:, i]) for i in range(2)])
            wo_bt = load_weight_blocks(kb, woT, "wo", wout_s, D, kb.wt[("wout", L)]) if kb.stage_mix else load_cast_weight(kb, woT, "wo", wout_s, D)
            wo_tiles = lambda d: [wo_bt[(d * 128) // 512]]
        xs_v = x_src.rearrange("(c p) t -> p c t", p=128)
        xd_v = x_dst.rearrange("(c p) t -> p c t", p=128)
        t_up = kb.wt[("up", L)]
        t_dn = kb.wt[("dn", L)]
        V = kb.VOFF
        vt = vec

        def load_tile(tt):
            xt = x_r.next()
            sc.op("sp", lambda e, xt=xt, tt=tt: e.dma_start(out=xt.ap, in_=xs_v[:, :, tt * TT:(tt + 1) * TT]),
                  reads=[xs_t[tt]], writes=[xt], dma=xt.name)
            ot = None
            if mix is not None:
                ot = o_r.next()
                ov = oT_d.rearrange("(c p) t -> p c t", p=128)
                sc.op("sp", lambda e, ot=ot, tt=tt: e.dma_start(out=ot.ap, in_=ov[:, :, tt * TT:(tt + 1) * TT]),
                      reads=[kb.od_t[tt]], writes=[ot], dma=ot.name)
            return xt, ot

        def preA(tt, xt, ot):
            if hook is not None:
                hook(tt)
            if mix is not None:
                for d in range(8):
                    yp = y_r.next()
                    for kc in range(KC):
                        sc.op("pe", lambda e, yp=yp, kc=kc, d=d: e.matmul(
                            yp.ap, woT[:, kc, d * 128:(d + 1) * 128], ot.ap[:, kc], start=(kc == 0), stop=(kc == KC - 1)),
                            reads=wo_tiles(d) + [ot], writes=[yp])
                    sc.op("dve", lambda e, yp=yp, d=d: e.tensor_tensor(
                        xt.ap[:, d], yp.ap, xt.ap[:, d], ALU.add), reads=[yp, xt], writes=[xt])
            sqs = []
            for c in range(8):
                sq = sq_r.next()
                sc.op("act", lambda e, sq=sq, c=c: e.activation(r32(sq.ap), xt.ap[:, c], AF.Square),
                      reads=[xt], writes=[sq])
                sqs.append(sq)
            return sqs

        def preB(tt, xt, sqs):
            for c in range(8):
                sq = sqs[c]
                sc.op("pe", lambda e, sq=sq, c=c: e.matmul(ss_ps.ap, r32(kb.ones32_t[:]), r32(sq.ap), start=(c == 0), stop=(c == 7)),
                      reads=[sq, kb.ones32], writes=[ss_ps])
            sc.op("act", lambda e: e.activation(lnT[:], ss_ps.ap, AF.Ln, bias=kb.eps_t[:], scale=1.0 / D),
                  reads=[ss_ps, kb.eps], writes=[ln_t])
            sc.op("act", lambda e: e.activation(rsT[:], lnT[:], AF.Exp, scale=-0.5), reads=[ln_t], writes=[rs_t])
            ht = h_r.next()
            for c in range(8):
                sc.op("dve", lambda e, c=c: e.scalar_tensor_tensor(
                    ht.ap[:, c], xt.ap[:, c], vt.ap[:, V["nf"](L) + c:V["nf"](L) + c + 1], rsT[:], ALU.mult, ALU.mult),
                    reads=[xt, rs_t, vt], writes=[ht])
            return ht

        def up(tt, xt, ht):
            for j in range(NJ):
                wu = wu_r.next()
                sc.op("sp", lambda e, wu=wu, j=j: e.dma_start(out=wu.ap, in_=wup_s[L, j]), reads=[t_up[j]],
                      writes=[wu], dma=wu.name)
                outs = []
                for gv in range(2):
                    pp = (gp_r if gv == 0 else vp_r).next()
                    for kc in range(8):
                        sc.op("pe", lambda e, pp=pp, wu=wu, gv=gv, kc=kc, ht=ht: e.matmul(
                            pp.ap, wu.ap[:, gv, kc, :], ht.ap[:, kc], start=(kc == 0), stop=(kc == 7)),
                            reads=[wu, ht], writes=[pp])
                    q = gv * NJ + j
                    dst = (g_r if gv == 0 else v_r).next()
                    cw = lambda tap, q=q: vt.ap[:, V["cw"](L) + tap * 2 * NJ + q:V["cw"](L) + tap * 2 * NJ + q + 1]
                    cb = vt.ap[:, V["cb"](L) + q:V["cb"](L) + q + 1]
                    sc.op("act", lambda e, dst=dst, pp=pp, cw=cw, cb=cb: e.activation(
                        dst.ap, pp.ap, AF.Identity, bias=cb, scale=cw(2)), reads=[pp, vt], writes=[dst])
                    sc.op("dve", lambda e, dst=dst, pp=pp, cw=cw: e.scalar_tensor_tensor(
                        dst.ap[:, 1:TT], pp.ap[:, 0:TT - 1], cw(1), dst.ap[:, 1:TT], ALU.mult, ALU.add),
                        reads=[pp, dst, vt], writes=[dst])
                    sc.op("dve", lambda e, dst=dst, pp=pp, cw=cw: e.scalar_tensor_tensor(
                        dst.ap[:, 2:TT], pp.ap[:, 0:TT - 2], cw(0), dst.ap[:, 2:TT], ALU.mult, ALU.add),
                        reads=[pp, dst, vt], writes=[dst])
                    hl = hl_t[tt % 2][q]
                    hln = hl_t[(tt + 1) % 2][q]
                    if tt + 1 < NT:
                        sc.op("act", lambda e, hln=hln, pp=pp: e.activation(hln.ap, pp.ap[:, TT - 2:TT], AF.Copy),
                              reads=[pp], writes=[hln])
                    if tt > 0:
                        sc.op("dve", lambda e, dst=dst, hl=hl, cw=cw: e.scalar_tensor_tensor(
                            dst.ap[:, 0:2], hl.ap[:, 0:2], cw(0), dst.ap[:, 0:2], ALU.mult, ALU.add),
                            reads=[hl, dst, vt], writes=[dst])
                        sc.op("dve", lambda e, dst=dst, hl=hl, cw=cw: e.scalar_tensor_tensor(
                            dst.ap[:, 0:1], hl.ap[:, 1:2], cw(1), dst.ap[:, 0:1], ALU.mult, ALU.add),
                            reads=[hl, dst, vt], writes=[dst])
                    outs.append(dst)
                gd, vd = outs
                sc.op("act", lambda e, gd=gd: e.activation(gd.ap, gd.ap, AF.Silu), reads=[gd], writes=[gd])
                sc.op("pool", lambda e, gd=gd, vd=vd, j=j: e.tensor_tensor(a_t[j].ap, gd.ap, vd.ap, ALU.mult),
                      reads=[gd, vd], writes=[a_t[j]])

        def down(tt, xt, d0, d1):
            for d in range(d0, d1):
                wd = wd_r.next()
                sc.op("sp", lambda e, wd=wd, d=d: e.dma_start(out=wd.ap, in_=wdn_s[L, d]), reads=[t_dn[d]],
                      writes=[wd], dma=wd.name)
                yp = y_r.next()
                for j in range(NJ):
                    sc.op("pe", lambda e, yp=yp, wd=wd, j=j: e.matmul(
                        yp.ap, wd.ap[:, j, :], a_t[j].ap, start=(j == 0), stop=(j == NJ - 1)),
                        reads=[wd, a_t[j]], writes=[yp])
                sc.op("dve", lambda e, yp=yp, d=d, xt=xt: e.tensor_tensor(
                    xt.ap[:, d], yp.ap, xt.ap[:, d], ALU.add), reads=[yp, xt], writes=[xt])
            if d1 < 8:
                return
            sc.op("act", lambda e, xt=xt, tt=tt: e.dma_start(out=xd_v[:, :, tt * TT:(tt + 1) * TT], in_=xt.ap),
                  reads=[xt], writes=[xd_t[tt]], dma="st_" + xt.name)

        tiles = {}
        tiles[0] = load_tile(0)
        hts = {}
        hts[0] = preB(0, tiles[0][0], preA(0, *tiles[0]))
        for tt in range(NT):
            up(tt, tiles[tt][0], hts[tt])
            if tt + 1 < NT:
                tiles[tt + 1] = load_tile(tt + 1)
                sqs = preA(tt + 1, *tiles[tt + 1])
                down(tt, tiles[tt][0], 0, 4)
                hts[tt + 1] = preB(tt + 1, tiles[tt + 1][0], sqs)
                down(tt, tiles[tt][0], 4, 8)
            else:
                down(tt, tiles[tt][0], 0, 8)
        kb.barrier()


ODD_IN = 4608
DILS = (1, 4, 16)


def load_cast_weight(kb, dst_t, name, src_v, ncols, step=512, first=()):
    nb = -(-ncols // step)
    tiles = [Tile("%s_b%d" % (name, b), None) for b in range(nb)]
    order = list(first) + [b for b in range(nb) if b not in first]
    for b in order:
        c0, c1 = b * step, min(ncols, (b + 1) * step)
        kb.sc.op("pool", lambda e, c0=c0, c1=c1: e.dma_start(out=dst_t[:, :, c0:c1], in_=src_v[:, :, c0:c1]),
                 writes=[tiles[b]], dma="w_" + name)
    return tiles


def load_weight_blocks(kb, dst_t, name, src_s, ncols, dep_tiles, step=512):
    tiles = []
    for bi, c0 in enumerate(range(0, ncols, step)):
        c1 = min(ncols, c0 + step)
        t = Tile("%s_b%d" % (name, bi), None)
        kb.sc.op("sp", lambda e, c0=c0, c1=c1: e.dma_start(out=dst_t[:, :, c0:c1], in_=src_s[:, :, c0:c1]),
                 reads=[dep_tiles[bi]], writes=[t], dma="w_" + name)
        tiles.append(t)
    return tiles


def prep_mix_weights(kb, key, src_v, dst_s, ncols, step=512):
    tiles = []
    th = []
    for bi, c0 in enumerate(range(0, ncols, step)):
        c1 = min(ncols, c0 + step)
        t = Tile("%s%d_p%d" % (key[0], key[1], bi), None)
        tiles.append(t)
        th.append(lambda c0=c0, c1=c1, t=t: kb.sc.op(
            "pool", lambda e: e.dma_start(out=dst_s[:, :, c0:c1], in_=src_v[:, :, c0:c1]), writes=[t], dma="wp_%s%d" % key))
    kb.wt[key] = tiles
    return th


def wblk(tiles, c0, c1, step=512):
    return tiles[c0 // step:(c1 - 1) // step + 1]


def norm_tile(kb, xt, ss_ps, sq_r, lnT, ln_t, rsT, rs_t):
    sc = kb.sc
    for c in range(8):
        sq = sq_r.next()
        sc.op("act", lambda e, sq=sq, c=c: e.activation(r32(sq.ap), xt.ap[:, c], AF.Square), reads=[xt], writes=[sq])
        sc.op("pe", lambda e, sq=sq, c=c: e.matmul(ss_ps.ap, r32(kb.ones32_t[:]), r32(sq.ap), start=(c == 0), stop=(c == 7)),
              reads=[sq, kb.ones32], writes=[ss_ps])
    sc.op("act", lambda e: e.activation(lnT[:], ss_ps.ap, AF.Ln, bias=kb.eps_t[:], scale=1.0 / D),
          reads=[ss_ps, kb.eps], writes=[ln_t])
    sc.op("act", lambda e: e.activation(rsT[:], lnT[:], AF.Exp, scale=-0.5), reads=[ln_t], writes=[rs_t])


def phase_odd_a(kb, L, Lo, x_src, xs_t, w_in, qk_s, v_s, vec, hook=None):
    nc, sc, S, NT = kb.nc, kb.sc, kb.S, kb.NT
    V = kb.VOFF
    with ExitStack() as pes:
        wT = kb.sb(pes, "oa_w", [128, 8, ODD_IN], BF16)
        w_bt = load_weight_blocks(kb, wT, "oa_w", w_in, ODD_IN, kb.wt[("win", L)]) if kb.stage_mix else load_cast_weight(kb, wT, "oa_w", w_in, ODD_IN)
        xT = kb.sb(pes, "oa_x", [128, 2, 8, TT], F32)
        h0T = kb.sb(pes, "oa_h0", [128, 2, 8, TT], BF16)
        h1T = kb.sb(pes, "oa_h1", [128, 2, 8, TT], BF16)
        h2T = kb.sb(pes, "oa_h2", [128, 8, 4 * TT], BF16)
        sqT = kb.sb(pes, "oa_sq", [128, 2, TT], F32)
        lnT = kb.sb(pes, "oa_ln", [128, TT], F32)
        rsT = kb.sb(pes, "oa_rs", [128, TT], F32)
        sq2T = kb.sb(pes, "oa_sq2", [128, 3, TT], F32)
        ln2T = kb.sb(pes, "oa_ln2", [128, 2, TT], F32)
        rs2T = kb.sb(pes, "oa_rs2", [128, 2, TT], F32)
        qoT = kb.sb(pes, "oa_qo", [128, 3, TT], BF16)
        voT = kb.sb(pes, "oa_vo", [128, 2, TT], BF16)
        x_r = Ring([Tile("x%d" % i, xT[:, i]) for i in range(2)])
        h0_r = Ring([[Tile("h0%d_%d" % (i, c), h0T[:, i, c]) for c in range(8)] for i in range(2)])
        h1_r = Ring([[Tile("h1%d_%d" % (i, c), h1T[:, i, c]) for c in range(8)] for i in range(2)])
        h2_t = [Tile("h2_%d" % c, h2T[:, c]) for c in range(8)]
        sq_r = Ring([Tile("sq%d" % i, sqT[:, i]) for i in range(2)])
        ln_t = Tile("ln", lnT[:])
        rs_t = Tile("rs", rsT[:])
        sq2_r = Ring([Tile("sq2%d" % i, sq2T[:, i]) for i in range(3)])
        ln2_r = Ring([Tile("ln2%d" % i, ln2T[:, i]) for i in range(2)])
        rs2_r = Ring([Tile("rs2%d" % i, rs2T[:, i]) for i in range(2)])
        qo_r = Ring([Tile("qo%d" % i, qoT[:, i]) for i in range(3)])
        vo_r = Ring([Tile("vo%d" % i, voT[:, i]) for i in range(2)])
        ps = kb.ptiles("oa")
        ss_ps = ps[0]
        z_r = Ring([ps[1], ps[2], ps[3]])
        s2_r = Ring([ps[4], ps[5]])
        vp_r = Ring([ps[6], ps[7]])
        xs_v = x_src.rearrange("(c p) t -> p c t", p=128)

        def load_tile(tt):
            xt = x_r.next()
            sc.op("sp", lambda e, xt=xt, tt=tt: e.dma_start(out=xt.ap, in_=xs_v[:, :, tt * TT:(tt + 1) * TT]),
                  reads=[xs_t[tt]], writes=[xt], dma=xt.name)
            return xt

        def do_proj(g, hv, hv_t, pos0, extra=None):
            def finish(zp, sq, qk, hh):
                gcol = V["oq"](Lo) + qk
                sp2 = s2_r.next()
                sc.op("pe", lambda e: e.matmul(sp2.ap, r32(kb.ones32_t[:]), r32(sq.ap), start=True, stop=True),
                      reads=[sq, kb.ones32], writes=[sp2])
                ln = ln2_r.next()
                sc.op("act", lambda e: e.activation(ln.ap, sp2.ap, AF.Ln, bias=kb.eps_t[:], scale=1.0 / 128),
                      reads=[sp2, kb.eps], writes=[ln])
                rs = rs2_r.next()
                sc.op("act", lambda e: e.activation(rs.ap, ln.ap, AF.Exp, scale=-0.5), reads=[ln], writes=[rs])
                qo = qo_r.next()
                sc.op("dve", lambda e: e.scalar_tensor_tensor(
                    qo.ap, zp.ap, vec.ap[:, gcol:gcol + 1], rs.ap, ALU.mult, ALU.mult),
                    reads=[zp, rs, vec], writes=[qo])
                sc.op("sp", lambda e: e.dma_start(
                    out=qk_s[g, qk, hh, :, pos0:pos0 + TT], in_=qo.ap), reads=[qo], writes=[kb.qk_t[g][qk][hh]], dma="st_" + qo.name)

            def vsub(sub):
                vp = vp_r.next()
                for kc in range(8):
                    sc.op("pe", lambda e, kc=kc: e.matmul(
                        vp.ap, hv[:, kc, sub * 128:(sub + 1) * 128], wT[:, kc, vcol:vcol + 512], start=(kc == 0), stop=(kc == 7)),
                        reads=wblk(w_bt, vcol, vcol + 512) + [hv_t[kc]], writes=[vp])
                vo = vo_r.next()
                sc.op("act", lambda e: e.activation(vo.ap, vp.ap, AF.Copy), reads=[vp], writes=[vo])
                sc.op("sp", lambda e: e.dma_start(
                    out=v_s[g, pos0 + sub * 128:pos0 + (sub + 1) * 128, :], in_=vo.ap), reads=[vo], writes=[kb.v_t[g]], dma="st_" + vo.name)

            vcol = ((g * 3 + 2) * 4) * 128
            pend = []
            for qk in range(2):
                for hh in range(4):
                    col = ((g * 3 + qk) * 4 + hh) * 128
                    zp = z_r.next()
                    for kc in range(8):
                        sc.op("pe", lambda e, zp=zp, kc=kc, col=col: e.matmul(
                            zp.ap, wT[:, kc, col:col + 128], hv[:, kc], start=(kc == 0), stop=(kc == 7)),
                            reads=wblk(w_bt, col, col + 128) + [hv_t[kc]], writes=[zp])
                    sq = sq2_r.next()
                    sc.op("act", lambda e, sq=sq, zp=zp: e.activation(r32(sq.ap), zp.ap, AF.Square), reads=[zp], writes=[sq])
                    pend.append((zp, sq, qk, hh))
                    if len(pend) > 1:
                        finish(*pend.pop(0))
                    if extra:
                        extra.pop(0)()
            vsub(0)
            finish(*pend.pop(0))
            for sub in range(1, 4):
                vsub(sub)

        def prep(tt, xt):
            if hook is not None:
                hook(tt)
            norm_tile(kb, xt, ss_ps, sq_r, lnT, ln_t, rsT, rs_t)
            h0 = h0_r.next()
            h1 = h1_r.next()
            s4 = tt % 4
            for c in range(8):
                gc = V["nm"](L) + c
                sc.op("dve", lambda e, c=c, gc=gc: e.scalar_tensor_tensor(
                    h0[c].ap, xt.ap[:, c], vec.ap[:, gc:gc + 1], rsT[:], ALU.mult, ALU.mult),
                    reads=[xt, rs_t, vec], writes=[h0[c]])
            for c in range(8):
                sc.op("pool", lambda e, c=c: e.tensor_copy(
                    h2T[:, c].rearrange("p (r i) -> p i r", r=16)[:, 32 * s4:32 * s4 + 32, :],
                    h0[c].ap.rearrange("p (i r) -> p i r", r=16)),
                    reads=[h0[c]], writes=[h2_t[c]])
            ex = []
            for c in range(8):
                gc = V["nm"](L) + c
                ex.append(lambda c=c, gc=gc: sc.op("dve", lambda e: e.scalar_tensor_tensor(
                    h1[c].ap.rearrange("p (r i) -> p i r", r=4), xt.ap[:, c].rearrange("p (i r) -> p i r", r=4),
                    vec.ap[:, gc:gc + 1], rsT[:].rearrange("p (i r) -> p i r", r=4), ALU.mult, ALU.mult),
                    reads=[xt, rs_t, vec], writes=[h1[c]]))
            return h0, h1, ex

        class _HV:
            def __init__(self, tiles):
                self.tiles = tiles

            def __getitem__(self, k):
                kc = k[1]
                ap = self.tiles[kc].ap
                return ap if len(k) == 2 else ap[:, k[2]]

        xts = {0: load_tile(0)}
        pr = {0: prep(0, xts[0])}
        for tt in range(NT):
            h0, h1, ex = pr[tt]
            if tt + 1 < NT:
                xts[tt + 1] = load_tile(tt + 1)
            do_proj(0, _HV(h0), h0, tt * TT, extra=ex)
            while ex:
                ex.pop(0)()
            if tt + 1 < NT and tt % 4 != 3:
                pr[tt + 1] = prep(tt + 1, xts[tt + 1])
            do_proj(1, _HV(h1), h1, tt * TT)
            if tt % 4 == 3:
                for s in range(4):
                    class _H2:
                        def __init__(self, s):
                            self.s = s

                        def __getitem__(self, k):
                            kc = k[1]
                            ap = h2T[:, kc, self.s * TT:(self.s + 1) * TT]
                            return ap if len(k) == 2 else ap[:, k[2]]
                    do_proj(2, _H2(s), h2_t, (tt // 4) * 4 * TT + s * TT)
                if tt + 1 < NT:
                    pr[tt + 1] = prep(tt + 1, xts[tt + 1])
        kb.barrier()


def phase_odd_b(kb, qk_s, v_s, od, thunks=None):
    nc, sc, S, NT = kb.nc, kb.sc, kb.S, kb.NT
    NB = S // 128
    with ExitStack() as pes:
        accO2 = kb.sb(pes, "ob_ao", [128, 2, S], F32)
        accD2 = kb.sb(pes, "ob_ad", [128, 2, S], F32)
        qT = kb.sb(pes, "ob_q", [128, 2, S], BF16)
        kT = kb.sb(pes, "ob_k", [128, 2, S], BF16)
        vT = kb.sb(pes, "ob_v", [128, 2, NB, 128], BF16)
        pT = kb.sb(pes, "ob_p", [128, 6, 512], BF16)
        rdT = kb.sb(pes, "ob_rd", [128, 2, 512], F32)
        ooT = kb.sb(pes, "ob_oo", [128, 2, 512], BF16)
        ao_t2 = [Tile("accO%d" % i, accO2[:, i]) for i in range(2)]
        ad_t2 = [Tile("accD%d" % i, accD2[:, i]) for i in range(2)]
        q_r = Ring([Tile("q%d" % i, qT[:, i]) for i in range(2)])
        k_r = Ring([Tile("k%d" % i, kT[:, i]) for i in range(2)])
        v_r = Ring([Tile("v%d" % i, vT[:, i]) for i in range(2)])
        p_r = Ring([Tile("p%d" % i, pT[:, i]) for i in range(6)])
        rd_r = Ring([Tile("rd%d" % i, rdT[:, i]) for i in range(2)])
        oo_r = Ring([Tile("oo%d" % i, ooT[:, i]) for i in range(2)])
        ps = kb.ptiles("ob")
        st_r = Ring([ps[0], ps[1], ps[2], ps[3]])
        o_r = Ring([ps[4], ps[5]])
        d_r = Ring([ps[6], ps[7]])
        sm = 128.0 ** -0.5
        LA = 3
        ld = {}
        cur = {}
        defer = []

        def load_hg(hh, g):
            if thunks:
                thunks.pop(0)()
            qt, kt, vt = q_r.next(), k_r.next(), v_r.next()
            sc.op("sp", lambda e: e.dma_start(out=qt.ap, in_=qk_s[g, 0, hh]),
                  reads=[kb.qk_t[g][0][hh]], writes=[qt], dma=qt.name)
            sc.op("sp", lambda e: e.dma_start(out=kt.ap, in_=qk_s[g, 1, hh]),
                  reads=[kb.qk_t[g][1][hh]], writes=[kt], dma=qt.name)
            sc.op("sp", lambda e: e.dma_start(
                out=vt.ap, in_=v_s[g].rearrange("(b p) c -> p b c", p=128)[:, :, hh * 128:(hh + 1) * 128]),
                reads=[kb.v_t[g]], writes=[vt], dma=qt.name)
            ld[(hh, g)] = (qt, kt, vt)

        def front(hh, g, pb4, half):
            if (hh, g) not in ld:
                load_hg(hh, g)
            qt, kt, vt = ld[(hh, g)]
            d = DILS[g]
            stp = st_r.next()
            pbs = (pb4 + 2 * half, pb4 + 2 * half + 1)
            for bi, pb in enumerate(pbs):
                hasprev = (pb // d) > 0
                pv = (pb - d) if hasprev else pb
                sc.op("pe", lambda e, bi=bi, pb=pb, pv=pv: e.matmul(
                    stp.ap[:, bi * 256:bi * 256 + 128], kt.ap[:, pv * 128:(pv + 1) * 128],
                    qt.ap[:, pb * 128:(pb + 1) * 128], start=True, stop=True), reads=[kt, qt], writes=[stp])
                sc.op("pe", lambda e, bi=bi, pb=pb: e.matmul(
                    stp.ap[:, bi * 256 + 128:bi * 256 + 256], kt.ap[:, pb * 128:(pb + 1) * 128],
                    qt.ap[:, pb * 128:(pb + 1) * 128], start=True, stop=True), reads=[kt, qt], writes=[stp])
            pt = p_r.next()
            sc.op("act", lambda e: e.activation(pt.ap, stp.ap, AF.Exp, scale=sm), reads=[stp], writes=[pt])
            sc.op("pool", lambda e: e.tensor_tensor(pt.ap, pt.ap, kb.omask_t[:], ALU.mult),
                  reads=[pt, kb.omask], writes=[pt])
            return pt

        def back(hh, g, pb4, half, pt):
            qt, kt, vt = ld[(hh, g)]
            d = DILS[g]
            if half == 0:
                cur["o"], cur["d"] = o_r.next(), d_r.next()
            op_, dp_ = cur["o"], cur["d"]
            pbs = (pb4 + 2 * half, pb4 + 2 * half + 1)
            for bi, pb in enumerate(pbs):
                hasprev = (pb // d) > 0
                slot = 2 * half + bi
                for (dst, isden) in ((op_, False), (dp_, True)):
                    if hasprev:
                        sc.op("pe", lambda e, dst=dst, isden=isden, slot=slot, pb=pb, bi=bi: e.matmul(
                            dst.ap[:, slot * 128:(slot + 1) * 128],
                            kb.onesb_t[:] if isden else vt.ap[:, pb - d, :],
                            pt.ap[:, bi * 256:bi * 256 + 128], start=True, stop=False),
                            reads=[pt, vt, kb.onesb], writes=[dst])
                    sc.op("pe", lambda e, dst=dst, isden=isden, slot=slot, pb=pb, bi=bi, hasprev=hasprev: e.matmul(
                        dst.ap[:, slot * 128:(slot + 1) * 128],
                        kb.onesb_t[:] if isden else vt.ap[:, pb, :],
                        pt.ap[:, bi * 256 + 128:bi * 256 + 256], start=(not hasprev), stop=True),
                        reads=[pt, vt, kb.onesb], writes=[dst])
            accO, accD, ao_t, ad_t = accO2[:, hh % 2], accD2[:, hh % 2], ao_t2[hh % 2], ad_t2[hh % 2]
            if half == 1:
                for (src, acc, acc_t) in ((op_, accO, ao_t), (dp_, accD, ad_t)):
                    if g == 0:
                        sc.op("act", lambda e, src=src, acc=acc: e.activation(
                            acc[:, pb4 * 128:(pb4 + 4) * 128], src.ap, AF.Copy), reads=[src], writes=[acc_t])
                    else:
                        if g == 1:
                            av = lambda acc=acc: acc[:, pb4 * 128:(pb4 + 4) * 128].rearrange("p (i r) -> p r i", r=4)
                        else:
                            n, r0 = pb4 // 16, pb4 % 16
                            av = lambda acc=acc, n=n, r0=r0: acc[:, n * 2048:(n + 1) * 2048].rearrange(
                                "p (i r) -> p r i", r=16)[:, r0:r0 + 4, :]
                        sc.op("dve", lambda e, src=src, av=av: e.tensor_tensor(
                            av(), src.ap.rearrange("p (r i) -> p r i", r=4), av(), ALU.add), reads=[src, acc_t], writes=[acc_t])
                if g == 2 and pb4 == NB - 4:
                    def fin(tt, accO=accO, accD=accD, ao_t=ao_t, ad_t=ad_t, hh=hh):
                        rd = rd_r.next()
                        oo = oo_r.next()
                        sc.op("act", lambda e: e.activation(rd.ap, accD[:, tt * TT:(tt + 1) * TT], AF.Ln), reads=[ad_t], writes=[rd])
                        sc.op("act", lambda e: e.activation(rd.ap, rd.ap, AF.Exp, scale=-1.0), reads=[rd], writes=[rd])
                        sc.op("dve", lambda e: e.tensor_tensor(oo.ap, accO[:, tt * TT:(tt + 1) * TT], rd.ap, ALU.mult),
                              reads=[ao_t, rd], writes=[oo])
                        sc.op("act", lambda e: e.dma_start(out=od[hh * 128:(hh + 1) * 128, tt * TT:(tt + 1) * TT], in_=oo.ap),
                              reads=[oo], writes=[kb.od_t[tt]], dma="st_" + oo.name)
                    for tt in range(NT):
                        defer.append(lambda tt=tt, fin=fin: fin(tt))

        steps = [(hh, g, pb4, half) for hh in range(4) for g in range(3) for pb4 in range(0, NB, 4) for half in range(2)]
        pend = []
        for st in steps:
            pend.append(st + (front(*st),))
            if len(pend) > LA:
                back(*pend.pop(0))
                if defer:
                    defer.pop(0)()
        while pend:
            back(*pend.pop(0))
        while defer:
            defer.pop(0)()
        while thunks:
            thunks.pop(0)()
        kb.barrier()


EVEN_IN = 3088
C_AQ, C_AK, C_AV, C_AG, C_AR, C_BQ, C_BK, C_BV = 0, 256, 512, 1024, 1536, 1552, 2064, 2576


def phase_even_a(kb, L, Le, x_src, xs_t, w_in, wa2_d, ba_d, sd, vec, hook=None):
    nc, sc, S, NT = kb.nc, kb.sc, kb.S, kb.NT
    V = kb.VOFF
    with ExitStack() as pes:
        wT = kb.sb(pes, "ea_w", [128, 8, EVEN_IN], BF16)
        w_bt = load_weight_blocks(kb, wT, "ea_w", w_in, EVEN_IN, kb.wt[("win", L)]) if kb.stage_mix else load_cast_weight(kb, wT, "ea_w", w_in, EVEN_IN, first=(3, 0, 1, 5))
        wa2T = kb.sb(pes, "ea_wa2", [16, 256], F32)
        baT = kb.sb(pes, "ea_ba", [1, 256], F32)
        wa_t = Tile("ea_wa", wa2T[:])
        sc.op("sp", lambda e: e.dma_start(out=wa2T[:], in_=wa2_d[Le]), writes=[wa_t], dma="ea_wa")
        sc.op("sp", lambda e: e.dma_start(out=baT[:], in_=ba_d[Le]), writes=[wa_t], dma="ea_wa")
        xT = kb.sb(pes, "ea_x", [128, 2, 8, TT], F32)
        h0T = kb.sb(pes, "ea_h0", [128, 2, 8, TT], BF16)
        sqT = kb.sb(pes, "ea_sq", [128, 2, TT], F32)
        lnT = kb.sb(pes, "ea_ln", [128, TT], F32)
        rsT = kb.sb(pes, "ea_rs", [128, TT], F32)
        sq2T = kb.sb(pes, "ea_sq2", [128, 2, TT], F32)
        ln2T = kb.sb(pes, "ea_ln2", [128, 2, TT], F32)
        rs2T = kb.sb(pes, "ea_rs2", [128, 2, TT], F32)
        arT = kb.sb(pes, "ea_ar", [16, TT], F32)
        e1T = kb.sb(pes, "ea_e1", [128, 2, 256], F32)
        spT = kb.sb(pes, "ea_sp", [128, 2, 256], F32)
        erT = kb.sb(pes, "ea_er", [128, 2, 256], F32)
        ebT = kb.sb(pes, "ea_eb", [128, 2, TT], F32)
        enbT = kb.sb(pes, "ea_enb", [128, 2, TT], F32)
        dcT = kb.sb(pes, "ea_dc", [128, 2, 8], F32)
        foT = kb.sb(pes, "ea_fo", [128, 4, TT], BF16)
        toT = kb.sb(pes, "ea_to", [128, 3, TT], BF16)
        keT = kb.sb(pes, "ea_ke", [128, 2, 256], BF16)
        x_r = Ring([Tile("x%d" % i, xT[:, i]) for i in range(2)])
        h0_r = Ring([Tile("h0%d" % i, h0T[:, i]) for i in range(2)])
        sq_r = Ring([Tile("sq%d" % i, sqT[:, i]) for i in range(2)])
        ln_t = Tile("ln", lnT[:])
        rs_t = Tile("rs", rsT[:])
        sq2_r = Ring([Tile("sq2%d" % i, sq2T[:, i]) for i in range(2)])
        ln2_r = Ring([Tile("ln2%d" % i, ln2T[:, i]) for i in range(2)])
        rs2_r = Ring([Tile("rs2%d" % i, rs2T[:, i]) for i in range(2)])
        ar_t = Tile("ar", arT[:])
        e1_r = Ring([Tile("e1%d" % i, e1T[:, i]) for i in range(2)])
        sp_r = Ring([Tile("sp%d" % i, spT[:, i]) for i in range(2)])
        er_r = Ring([Tile("er%d" % i, erT[:, i]) for i in range(2)])
        eb_t = [Tile("eb%d" % i, ebT[:, i]) for i in range(2)]
        enb_t = [Tile("enb%d" % i, enbT[:, i]) for i in range(2)]
        dc_r = Ring([Tile("dc%d" % i, dcT[:, i]) for i in range(2)])
        fo_r = Ring([Tile("fo%d" % i, foT[:, i]) for i in range(4)])
        to_r = Ring([Tile("to%d" % i, toT[:, i]) for i in range(3)])
        ke_r = Ring([Tile("ke%d" % i, keT[:, i]) for i in range(2)])
        ps = kb.ptiles("ea")
        z_r = Ring([ps[1], ps[2], ps[0]])
        bt_ps = [ps[3], ps[4]]
        s_r = Ring([ps[5], ps[6]])
        tk_r = Ring([ps[7]])
        xs_v = x_src.rearrange("(c p) t -> p c t", p=128)

        def load_tile(tt):
            xt = x_r.next()
            sc.op("sp", lambda e, xt=xt, tt=tt: e.dma_start(out=xt.ap, in_=xs_v[:, :, tt * TT:(tt + 1) * TT]),
                  reads=[xs_t[tt]], writes=[xt], dma=xt.name)
            return xt

        def fm_proj(h0, col, M=128):
            zp = z_r.next()
            for kc in range(8):
                sc.op("pe", lambda e, zp=zp, kc=kc, h0=h0: e.matmul(
                    zp.ap[0:M, :], wT[:, kc, col:col + M], h0.ap[:, kc], start=(kc == 0), stop=(kc == 7)),
                    reads=wblk(w_bt, col, col + M) + [h0], writes=[zp])
            return zp

        def store_fm(fo, dst_ap, dst_tile):
            sc.op("sp", lambda e: e.dma_start(out=dst_ap, in_=fo.ap), reads=[fo], writes=[dst_tile], dma="st_" + fo.name)

        nxt = load_tile(0)
        for tt in range(NT):
            xt = nxt
            if tt + 1 < NT:
                nxt = load_tile(tt + 1)
            t0, t1 = tt * TT, (tt + 1) * TT
            if hook is not None:
                hook(tt)
            ss_ps = s_r.next()
            norm_tile(kb, xt, ss_ps, sq_r, lnT, ln_t, rsT, rs_t)
            h0 = h0_r.next()
            for c in range(8):
                gc = V["nm"](L) + c
                sc.op("dve", lambda e, c=c, gc=gc, h0=h0, xt=xt: e.scalar_tensor_tensor(
                    h0.ap[:, c], xt.ap[:, c], vec.ap[:, gc:gc + 1], rsT[:], ALU.mult, ALU.mult),
                    reads=[xt, rs_t, vec], writes=[h0])
            zp = fm_proj(h0, C_AR, M=16)
            sc.op("act", lambda e, zp=zp: e.activation(arT[:], zp.ap[0:16, :], AF.Copy), reads=[zp], writes=[ar_t])
            for sub in range(4):
                c0, c1 = sub * 128, (sub + 1) * 128
                la = s_r.next()
                sc.op("pe", lambda e, la=la, c0=c0, c1=c1: e.matmul(la.ap[:, 0:256], arT[0:16, c0:c1], wa2T[:], start=True, stop=False),
                      reads=[ar_t, wa_t], writes=[la])
                sc.op("pe", lambda e, la=la: e.matmul(la.ap[:, 0:256], kb.ones32_t[0:1, :], baT[:], start=False, stop=True),
                      reads=[kb.ones32, wa_t], writes=[la])
                e1 = e1_r.next()
                sc.op("act", lambda e, e1=e1, la=la: e.activation(e1.ap, la.ap[:, 0:256], AF.Exp, scale=-1.0), reads=[la], writes=[e1])
                sp = sp_r.next()
                sc.op("act", lambda e, e1=e1, sp=sp: e.activation(sp.ap, e1.ap, AF.Ln, bias=kb.one_t[:], scale=1.0),
                      reads=[e1, kb.one], writes=[sp])
                for fc in range(2):
                    sc.op("pe", lambda e, fc=fc, sp=sp, c0=c0, c1=c1: e.matmul(
                        bt_ps[fc].ap[:, c0:c1], sp.ap[:, fc * 128:(fc + 1) * 128], kb.umask_t[:], start=True, stop=True),
                        reads=[sp, kb.gmasks], writes=[bt_ps[fc]])
                br = s_r.next()
                sc.op("pe", lambda e, br=br, sp=sp: e.matmul(br.ap[:, 0:256], kb.lmask_t[:], sp.ap, start=True, stop=True),
                      reads=[sp, kb.gmasks], writes=[br])
                er = er_r.next()
                sc.op("act", lambda e, er=er, br=br: e.activation(er.ap, br.ap[:, 0:256], AF.Exp), reads=[br], writes=[er])
                for (col, ncol, kind) in ((C_AK, 256, "ke"), (C_AV, 512, "av"), (C_BV, 512, "bv")):
                    tp = tk_r.next()
                    for kc in range(8):
                        sc.op("pe", lambda e, tp=tp, kc=kc, c0=c0, c1=c1, col=col, ncol=ncol, h0=h0: e.matmul(
                            tp.ap[:, 0:ncol], h0.ap[:, kc, c0:c1], wT[:, kc, col:col + ncol], start=(kc == 0), stop=(kc == 7)),
                            reads=wblk(w_bt, col, col + ncol) + [h0], writes=[tp])
                    if kind == "ke":
                        ke = ke_r.next()
                        sc.op("dve", lambda e, ke=ke, tp=tp, er=er: e.tensor_tensor(ke.ap, tp.ap[:, 0:256], er.ap, ALU.mult),
                              reads=[tp, er], writes=[ke])
                        sc.op("sp", lambda e, ke=ke, c0=c0, c1=c1, t0=t0: e.dma_start(out=sd["gke"][t0 + c0:t0 + c1, :], in_=ke.ap),
                              reads=[ke], writes=[kb.sd_t["gke"]], dma="st_" + ke.name)
                    else:
                        to = to_r.next()
                        sc.op("act", lambda e, to=to, tp=tp: e.activation(to.ap, tp.ap, AF.Copy), reads=[tp], writes=[to])
                        nm = "gv" if kind == "av" else "dv"
                        sc.op("sp", lambda e, to=to, c0=c0, c1=c1, t0=t0, nm=nm: e.dma_start(out=sd[nm][t0 + c0:t0 + c1, :], in_=to.ap),
                              reads=[to], writes=[kb.sd_t[nm]], dma="st_" + to.name)
            for fc in range(2):
                sc.op("act", lambda e, fc=fc: e.activation(ebT[:, fc], bt_ps[fc].ap, AF.Exp), reads=[bt_ps[fc]], writes=[eb_t[fc]])
                sc.op("act", lambda e, fc=fc: e.activation(enbT[:, fc], bt_ps[fc].ap, AF.Exp, scale=-1.0), reads=[bt_ps[fc]], writes=[enb_t[fc]])
                dc = dc_r.next()
                sc.op("dve", lambda e, fc=fc, dc=dc: e.tensor_copy(dc.ap, ebT[:, fc].rearrange("p (n c) -> p n c", c=64)[:, :, 63]),
                      reads=[eb_t[fc]], writes=[dc])
                sc.op("sp", lambda e, fc=fc, dc=dc, tt=tt: e.dma_start(out=sd["gdec"][fc * 128:(fc + 1) * 128, tt * 8:(tt + 1) * 8], in_=dc.ap),
                      reads=[dc], writes=[kb.sd_t["gdec"]], dma="st_" + dc.name)
            for fc in range(2):
                zp = fm_proj(h0, C_AQ + fc * 128)
                fo = fo_r.next()
                sc.op("dve", lambda e, zp=zp, fo=fo, fc=fc: e.scalar_tensor_tensor(fo.ap, zp.ap, 0.125, ebT[:, fc], ALU.mult, ALU.mult),
                      reads=[zp, eb_t[fc]], writes=[fo])
                store_fm(fo, sd["gq"][fc * 128:(fc + 1) * 128, t0:t1], kb.sd_t["gq"])
                zp = fm_proj(h0, C_AK + fc * 128)
                fo = fo_r.next()
                sc.op("dve", lambda e, zp=zp, fo=fo, fc=fc: e.tensor_tensor(fo.ap, zp.ap, enbT[:, fc], ALU.mult),
                      reads=[zp, enb_t[fc]], writes=[fo])
                store_fm(fo, sd["gk"][fc * 128:(fc + 1) * 128, t0:t1], kb.sd_t["gk"])
            for c in range(4):
                zp = fm_proj(h0, C_AG + c * 128)
                fo = fo_r.next()
                sc.op("act", lambda e, zp=zp, fo=fo: e.activation(fo.ap, zp.ap, AF.Silu), reads=[zp], writes=[fo])
                store_fm(fo, sd["gg"][c * 128:(c + 1) * 128, t0:t1], kb.sd_t["gg"])
            def finish_f(zp, sq, gcol, nm, c):
                sp2 = s_r.next()
                sc.op("pe", lambda e: e.matmul(sp2.ap, r32(kb.bd64r_t[:]), r32(sq.ap), start=True, stop=True),
                      reads=[sq, kb.gmasks], writes=[sp2])
                ln = ln2_r.next()
                sc.op("act", lambda e: e.activation(ln.ap, sp2.ap, AF.Ln, bias=kb.eps_t[:], scale=1.0 / 64),
                      reads=[sp2, kb.eps], writes=[ln])
                rs = rs2_r.next()
                sc.op("act", lambda e: e.activation(rs.ap, ln.ap, AF.Exp, scale=-0.5), reads=[ln], writes=[rs])
                fo = fo_r.next()
                sc.op("dve", lambda e: e.scalar_tensor_tensor(
                    fo.ap, zp.ap, vec.ap[:, gcol:gcol + 1], rs.ap, ALU.mult, ALU.mult), reads=[zp, rs, vec], writes=[fo])
                store_fm(fo, sd[nm][c * 128:(c + 1) * 128, t0:t1], kb.sd_t[nm])

            pend = []
            for (cbase, gcol, nm) in ((C_BQ, V["eg"](Le) + 1, "dq"), (C_BK, V["eg"](Le) + 2, "dk")):
                for c in range(4):
                    zp = fm_proj(h0, cbase + c * 128)
                    sq = sq2_r.next()
                    sc.op("act", lambda e, sq=sq, zp=zp: e.activation(r32(sq.ap), zp.ap, AF.Square), reads=[zp], writes=[sq])
                    pend.append((zp, sq, gcol, nm, c))
                    if len(pend) > 1:
                        finish_f(*pend.pop(0))
            finish_f(*pend.pop(0))
        kb.barrier()


def head_norm_store(kb, src_ap, src_tiles, sq_r, ss_ps, ln_r, rs_r, gain_ap, gain_tiles, mul_tile, out_r, dst_ap, dst_tile, tmp_r=None):
    sc = kb.sc
    sq = sq_r.next()
    sc.op("act", lambda e: e.activation(r32(sq.ap), src_ap, AF.Square), reads=src_tiles, writes=[sq])
    sc.op("pe", lambda e: e.matmul(ss_ps.ap, r32(kb.ones32_t[:]), r32(sq.ap), start=True, stop=True), reads=[sq, kb.ones32], writes=[ss_ps])
    ln = ln_r.next()
    sc.op("act", lambda e: e.activation(ln.ap, ss_ps.ap, AF.Ln, bias=kb.eps_t[:], scale=1.0 / 128), reads=[ss_ps, kb.eps], writes=[ln])
    rs = rs_r.next()
    sc.op("act", lambda e: e.activation(rs.ap, ln.ap, AF.Exp, scale=-0.5), reads=[ln], writes=[rs])
    oo = out_r.next()
    if mul_tile is None:
        sc.op("dve", lambda e: e.scalar_tensor_tensor(oo.ap, src_ap, gain_ap, rs.ap, ALU.mult, ALU.mult),
              reads=src_tiles + [rs] + gain_tiles, writes=[oo])
    else:
        tmp = tmp_r.next()
        sc.op("dve", lambda e: e.scalar_tensor_tensor(tmp.ap, src_ap, gain_ap, rs.ap, ALU.mult, ALU.mult),
              reads=src_tiles + [rs] + gain_tiles, writes=[tmp])
        sc.op("pool", lambda e: e.tensor_tensor(oo.ap, tmp.ap, mul_tile.ap, ALU.mult), reads=[tmp, mul_tile], writes=[oo])
    sc.op("act", lambda e: e.dma_start(out=dst_ap, in_=oo.ap), reads=[oo], writes=[dst_tile], dma="st_" + oo.name)


def phase_even_gla(kb, Le, sd, od, vec):
    nc, sc, S, NT = kb.nc, kb.sc, kb.S, kb.NT
    NB = S // 128
    V = kb.VOFF
    with ExitStack() as pes:
        gqT = kb.sb(pes, "eg_q", [128, 2, S], BF16)
        gkT = kb.sb(pes, "eg_k", [128, 2, S], BF16)
        gkeT = kb.sb(pes, "eg_ke", [128, NB, 256], BF16)
        gvT = kb.sb(pes, "eg_v", [128, NB, 512], BF16)
        decT = kb.sb(pes, "eg_dec", [128, 2, S // 64], F32)
        ggT = kb.sb(pes, "eg_gg", [128, 3, TT], BF16)
        s32T = kb.sb(pes, "eg_s32", [128, 2, 128], F32)
        sbfT = kb.sb(pes, "eg_sbf", [128, 2, 4, 128], BF16)
        attT = kb.sb(pes, "eg_att", [128, 4, 128], BF16)
        sqT = kb.sb(pes, "eg_sq", [128, 2, TT], F32)
        lnT = kb.sb(pes, "eg_ln", [128, 2, TT], F32)
        rsT = kb.sb(pes, "eg_rs", [128, 2, TT], F32)
        tmT = kb.sb(pes, "eg_tm", [128, 2, TT], F32)
        ooT = kb.sb(pes, "eg_oo", [128, 3, TT], BF16)
        in_t = Tile("eg_in", None)
        for fc in range(2):
            sc.op("sp", lambda e, fc=fc: e.dma_start(out=decT[:, fc], in_=sd["gdec"][fc * 128:(fc + 1) * 128, :]),
                  reads=[kb.sd_t["gdec"]], writes=[in_t], dma="eg_in")
        NP = 4
        in_p = [Tile("eg_in%d" % i, None) for i in range(NP)]
        bpp = NB // NP
        for i in range(NP):
            b0, b1 = i * bpp, (i + 1) * bpp
            for fc in range(2):
                sc.op("sp", lambda e, fc=fc, b0=b0, b1=b1: e.dma_start(
                    out=gqT[:, fc, b0 * 128:b1 * 128], in_=sd["gq"][fc * 128:(fc + 1) * 128, b0 * 128:b1 * 128]),
                    reads=[kb.sd_t["gq"]], writes=[in_p[i]], dma="eg_in%d" % i)
                sc.op("sp", lambda e, fc=fc, b0=b0, b1=b1: e.dma_start(
                    out=gkT[:, fc, b0 * 128:b1 * 128], in_=sd["gk"][fc * 128:(fc + 1) * 128, b0 * 128:b1 * 128]),
                    reads=[kb.sd_t["gk"]], writes=[in_p[i]], dma="eg_in%d" % i)
            sc.op("sp", lambda e, b0=b0, b1=b1: e.dma_start(
                out=gkeT[:, b0:b1], in_=sd["gke"].rearrange("(b p) c -> p b c", p=128)[:, b0:b1]),
                reads=[kb.sd_t["gke"]], writes=[in_p[i]], dma="eg_in%d" % i)
            sc.op("sp", lambda e, b0=b0, b1=b1: e.dma_start(
                out=gvT[:, b0:b1], in_=sd["gv"].rearrange("(b p) c -> p b c", p=128)[:, b0:b1]),
                reads=[kb.sd_t["gv"]], writes=[in_p[i]], dma="eg_in%d" % i)
        gg_r = Ring([Tile("gg%d" % i, ggT[:, i]) for i in range(3)])
        s32_t = [Tile("s32%d" % i, s32T[:, i]) for i in range(2)]
        sbf_r = [Ring([Tile("sbf%d_%d" % (p, i), sbfT[:, p, i]) for i in range(4)]) for p in range(2)]
        att_r = Ring([Tile("att%d" % i, attT[:, i]) for i in range(4)])
        sq_r = Ring([Tile("sq%d" % i, sqT[:, i]) for i in range(2)])
        ln_r = Ring([Tile("ln%d" % i, lnT[:, i]) for i in range(2)])
        rs_r = Ring([Tile("rs%d" % i, rsT[:, i]) for i in range(2)])
        tm_r = Ring([Tile("tm%d" % i, tmT[:, i]) for i in range(2)])
        oo_r = Ring([Tile("oo%d" % i, ooT[:, i]) for i in range(3)])
        oeT = kb.sb(pes, "eg_oe", [128, 4, TT], F32)
        oe_r = Ring([Tile("oe%d" % i, oeT[:, i]) for i in range(4)])
        ps = kb.ptiles("eg")
        o_ps = [ps[0], ps[1], ps[2], ps[3]]
        at_b = [ps[4], ps[5]]
        kv_b = [ps[6], ps[7]]
        ss_ps = ps[4]
        cur = []
        for p in range(2):
            sc.op("pool", lambda e, p=p: e.memset(s32T[:, p], 0.0), writes=[s32_t[p]])
            sb0 = sbf_r[p].next()
            sc.op("pool", lambda e, sb0=sb0: e.memset(sb0.ap, 0.0), writes=[sb0])
            cur.append(sb0)
        for tt in range(NT):
            for sub in range(4):
                sbk = tt * 4 + sub
                k0, k1 = sbk * 128, (sbk + 1) * 128
                atts = []
                for h in range(4):
                    fc, po = h // 2, (h % 2) * 64
                    ap_ = at_b[h % 2]
                    sc.op("pe", lambda e, ap_=ap_, fc=fc, po=po, k0=k0, k1=k1: e.matmul(
                        ap_.ap[:, fc * 128:(fc + 1) * 128], gkT[po:po + 64, fc, k0:k1], gqT[po:po + 64, fc, k0:k1], start=True, stop=True),
                        reads=[in_p[sbk // bpp]], writes=[ap_])
                    at = att_r.next()
                    sc.op("dve", lambda e, at=at, ap_=ap_, fc=fc: e.tensor_tensor(at.ap, ap_.ap[:, fc * 128:(fc + 1) * 128], kb.gmask_t[:], ALU.mult),
                          reads=[ap_, kb.gmasks], writes=[at])
                    atts.append(at)
                s_c1 = []
                nxt_state = []
                for p in range(2):
                    sts = [cur[p]]
                    for c in range(2):
                        for hp in range(2):
                            h = p * 2 + hp
                            po = hp * 64
                            sc.op("pe", lambda e, c=c, h=h, po=po, sbk=sbk, p=p: e.matmul(
                                kv_b[c].ap[po:po + 64, p * 128:(p + 1) * 128], gkeT[c * 64:(c + 1) * 64, sbk, h * 64:(h + 1) * 64],
                                gvT[c * 64:(c + 1) * 64, sbk, h * 128:(h + 1) * 128], start=True, stop=True),
                                reads=[in_p[sbk // bpp]], writes=[kv_b[c]])
                        ch = sbk * 2 + c
                        sc.op("dve", lambda e, p=p, c=c, ch=ch: e.scalar_tensor_tensor(
                            s32T[:, p], s32T[:, p], decT[:, p, ch:ch + 1], kv_b[c].ap[:, p * 128:(p + 1) * 128], ALU.mult, ALU.add),
                            reads=[s32_t[p], kv_b[c], in_t], writes=[s32_t[p]])
                        sbn = sbf_r[p].next()
                        sc.op("act", lambda e, p=p, sbn=sbn: e.activation(sbn.ap, s32T[:, p], AF.Copy), reads=[s32_t[p]], writes=[sbn])
                        sts.append(sbn)
                    s_c1.append(sts)
                    nxt_state.append(sts[2])
                for h in range(4):
                    fc, po, p = h // 2, (h % 2) * 64, h // 2
                    st0, st1 = s_c1[p][0], s_c1[p][1]
                    at = atts[h]
                    oc0 = sub * 128
                    sc.op("pe", lambda e, h=h, at=at, sbk=sbk, oc0=oc0: e.matmul(
                        o_ps[h].ap[:, oc0:oc0 + 128], gvT[:, sbk, h * 128:(h + 1) * 128], at.ap, start=True, stop=False),
                        reads=[in_p[sbk // bpp], at], writes=[o_ps[h]])
                    sc.op("pe", lambda e, h=h, st0=st0, fc=fc, po=po, k0=k0, oc0=oc0: e.matmul(
                        o_ps[h].ap[:, oc0:oc0 + 64], st0.ap[po:po + 64, :], gqT[po:po + 64, fc, k0:k0 + 64], start=False, stop=False),
                        reads=[in_p[sbk // bpp], st0], writes=[o_ps[h]])
                    sc.op("pe", lambda e, h=h, st1=st1, fc=fc, po=po, k0=k0, oc0=oc0: e.matmul(
                        o_ps[h].ap[:, oc0 + 64:oc0 + 128], st1.ap[po:po + 64, :], gqT[po:po + 64, fc, k0 + 64:k0 + 128], start=False, stop=True),
                        reads=[in_p[sbk // bpp], st1], writes=[o_ps[h]])
                cur = nxt_state
            for h in range(4):
                gg = gg_r.next()
                sc.op("sp", lambda e, gg=gg, h=h, tt=tt: e.dma_start(out=gg.ap, in_=sd["gg"][h * 128:(h + 1) * 128, tt * TT:(tt + 1) * TT]),
                      reads=[kb.sd_t["gg"]], writes=[gg], dma=gg.name)
                gcol = V["eg"](Le)
                oe = oe_r.next()
                sc.op("act", lambda e, oe=oe, h=h: e.activation(oe.ap, o_ps[h].ap, AF.Copy), reads=[o_ps[h]], writes=[oe])
                head_norm_store(kb, oe.ap, [oe], sq_r, at_b[0], ln_r, rs_r, vec.ap[:, gcol:gcol + 1], [vec], gg, oo_r,
                                od[h * 128:(h + 1) * 128, tt * TT:(tt + 1) * TT], kb.od_t[tt], tmp_r=tm_r)
        kb.barrier()


def phase_even_diff(kb, L, Le, sd, od, vec, lvec_d, thunks=None):
    nc, sc, S, NT = kb.nc, kb.sc, kb.S, kb.NT
    NB = S // 128
    V = kb.VOFF
    lam_init = 0.8 - 0.6 * float(np.exp(-0.3 * L))
    with ExitStack() as pes:
        dqT = kb.sb(pes, "ed_q", [128, 2, 2, S], BF16)
        dkT = kb.sb(pes, "ed_k", [128, 2, S], BF16)
        dvT = kb.sb(pes, "ed_v", [128, 2, NB, 128], BF16)
        pT = kb.sb(pes, "ed_p", [128, 8, TT], BF16)
        lvT = kb.sb(pes, "ed_lv", [128, 4, 64], F32)
        ltT = kb.sb(pes, "ed_lt", [128, 2, 64], F32)
        lsT = kb.sb(pes, "ed_ls", [128, 8], F32)
        rT = kb.sb(pes, "ed_r", [128, 2, TT], F32)
        t1T = kb.sb(pes, "ed_t1", [128, 2, TT], F32)
        oT = kb.sb(pes, "ed_o", [128, 2, TT], F32)
        sqT = kb.sb(pes, "ed_sq", [128, 2, TT], F32)
        lnT = kb.sb(pes, "ed_ln", [128, 2, TT], F32)
        rsT = kb.sb(pes, "ed_rs", [128, 2, TT], F32)
        ooT = kb.sb(pes, "ed_oo", [128, 3, TT], BF16)
        q_r = Ring([Tile("q%d" % i, dqT[:, i]) for i in range(2)])
        for i_ in range(2):
            sc.op("pool", lambda e, i_=i_: e.memset(dqT[64:128, i_, 0, :], 0.0), writes=[q_r.tiles[i_]])
            sc.op("pool", lambda e, i_=i_: e.memset(dqT[0:64, i_, 1, :], 0.0), writes=[q_r.tiles[i_]])
        k_r = Ring([Tile("k%d" % i, dkT[:, i]) for i in range(2)])
        v_r = Ring([Tile("v%d" % i, dvT[:, i]) for i in range(2)])
        p_r = Ring([Tile("p%d" % i, pT[:, i]) for i in range(8)])
        r_r = Ring([Tile("r%d" % i, rT[:, i]) for i in range(2)])
        t1_r = Ring([Tile("t1%d" % i, t1T[:, i]) for i in range(2)])
        o_r = Ring([Tile("o%d" % i, oT[:, i]) for i in range(2)])
        sq_r = Ring([Tile("sq%d" % i, sqT[:, i]) for i in range(2)])
        ln_r = Ring([Tile("ln%d" % i, lnT[:, i]) for i in range(2)])
        rs_r = Ring([Tile("rs%d" % i, rsT[:, i]) for i in range(2)])
        oo_r = Ring([Tile("oo%d" % i, ooT[:, i]) for i in range(3)])
        ps = kb.ptiles("ed")
        st_r = Ring([ps[0], ps[1], ps[2], ps[3]])
        acc = [[ps[4], ps[5]], [ps[6], ps[7]]]
        lv_t = Tile("lv", lvT[:])
        ls_t = Tile("ls", lsT[:])
        sc.op("sp", lambda e: e.dma_start(out=lvT[:], in_=lvec_d[Le]), writes=[lv_t], dma="ed_lv")
        for i in range(2):
            sc.op("dve", lambda e, i=i: e.tensor_tensor(ltT[:, i], lvT[:, 2 * i], lvT[:, 2 * i + 1], ALU.mult), reads=[lv_t], writes=[ls_t])
            sc.op("dve", lambda e, i=i: e.reduce_sum(lsT[:, i:i + 1], ltT[:, i], AX.X), reads=[ls_t], writes=[ls_t])
            sc.op("act", lambda e, i=i: e.activation(lsT[:, 2 + i:3 + i], lsT[:, i:i + 1], AF.Exp), reads=[ls_t], writes=[ls_t])
        sc.op("dve", lambda e: e.tensor_tensor(lsT[:, 4:5], lsT[:, 3:4], lsT[:, 2:3], ALU.subtract), reads=[ls_t], writes=[ls_t])
        sc.op("dve", lambda e: e.tensor_scalar(lsT[:, 4:5], lsT[:, 4:5], -lam_init, None, ALU.add), reads=[ls_t], writes=[ls_t])
        gcol = V["eg"](Le) + 3
        sc.op("dve", lambda e: e.tensor_scalar(lsT[:, 5:6], vec.ap[:, gcol:gcol + 1], 1.0 - lam_init, None, ALU.mult),
              reads=[ls_t, vec], writes=[ls_t])
        LA = 4
        hd = {}

        def load_head(h):
            qt, kt, vt = q_r.next(), k_r.next(), v_r.next()
            sc.op("sp", lambda e: e.dma_start(out=qt.ap[0:64, 0, :], in_=sd["dq"][h * 128:h * 128 + 64, :]),
                  reads=[kb.sd_t["dq"]], writes=[qt], dma=qt.name)
            sc.op("sp", lambda e: e.dma_start(out=qt.ap[64:128, 1, :], in_=sd["dq"][h * 128 + 64:(h + 1) * 128, :]),
                  reads=[kb.sd_t["dq"]], writes=[qt], dma=qt.name)
            sc.op("sp", lambda e: e.dma_start(out=kt.ap, in_=sd["dk"][h * 128:(h + 1) * 128, :]),
                  reads=[kb.sd_t["dk"]], writes=[kt], dma=qt.name)
            sc.op("sp", lambda e: e.dma_start(
                out=vt.ap, in_=sd["dv"].rearrange("(b p) c -> p b c", p=128)[:, :, h * 128:(h + 1) * 128]),
                reads=[kb.sd_t["dv"]], writes=[vt], dma=qt.name)
            hd[h] = (qt, kt, vt)

        def front(h, qi, kbi, c):
            if h not in hd:
                load_head(h)
            qt, kt, vt = hd[h]
            q0, q1 = qi * TT, (qi + 1) * TT
            k0, k1 = kbi * 128, (kbi + 1) * 128
            stp = st_r.next()
            sc.op("pe", lambda e: e.matmul(
                stp.ap, kt.ap[:, k0:k1], qt.ap[:, c, q0:q1], start=True, stop=True),
                reads=[kt, qt], writes=[stp])
            pt = p_r.next()
            sc.op("act", lambda e: e.activation(pt.ap, stp.ap, AF.Exp, scale=0.125), reads=[stp], writes=[pt])
            dg = kbi - 4 * qi
            if dg >= 0:
                sc.op("pool", lambda e: e.tensor_tensor(pt.ap, pt.ap, kb.dmask_t[:, dg], ALU.mult),
                      reads=[pt, kb.dmask], writes=[pt])
            return pt

        def back(h, qi, kbi, c, pt):
            qt, kt, vt = hd[h]
            nkb = 4 * qi + 4
            sc.op("pe", lambda e: e.matmul(
                acc[c][0].ap, vt.ap[:, kbi, :], pt.ap, start=(kbi == 0), stop=(kbi == nkb - 1)),
                reads=[vt, pt], writes=[acc[c][0]])
            sc.op("pe", lambda e: e.matmul(
                acc[c][1].ap, kb.onesb_t[:], pt.ap, start=(kbi == 0), stop=(kbi == nkb - 1)),
                reads=[kb.onesb, pt], writes=[acc[c][1]])
            if kbi == nkb - 1 and c == 1:
                epilogue(h, qi)

        def epilogue(h, qi):
            q0, q1 = qi * TT, (qi + 1) * TT
            if thunks:
                thunks.pop(0)()
            ts_ = []
            evs = []
            for c in range(2):
                r = r_r.next()
                sc.op("act", lambda e, r=r, c=c: e.activation(r.ap, acc[c][1].ap, AF.Ln), reads=[acc[c][1]], writes=[r])
                t1 = t1_r.next()
                sc.op("dve", lambda e, t1=t1, c=c: e.tensor_copy(t1.ap, acc[c][0].ap), reads=[acc[c][0]], writes=[t1])
                evs.append((r, t1))
            for c in range(2):
                r, t1 = evs[c]
                sc.op("act", lambda e, r=r: e.activation(r.ap, r.ap, AF.Exp, scale=-1.0), reads=[r], writes=[r])
                sc.op("dve", lambda e, r=r, t1=t1: e.tensor_tensor(t1.ap, t1.ap, r.ap, ALU.mult), reads=[t1, r], writes=[t1])
                ts_.append(t1)
            o = o_r.next()
            sc.op("dve", lambda e, o=o, ta=ts_[0], tb=ts_[1]: e.scalar_tensor_tensor(o.ap, tb.ap, lsT[:, 4:5], ta.ap, ALU.mult, ALU.add),
                  reads=[ts_[0], ts_[1], ls_t], writes=[o])
            ssp = st_r.next()
            head_norm_store(kb, o.ap, [o], sq_r, ssp, ln_r, rs_r, lsT[:, 5:6], [ls_t], None, oo_r,
                            od[512 + h * 128:512 + (h + 1) * 128, q0:q1], kb.od_t[qi])

        steps = [(h, qi, kbi, c) for h in range(4) for qi in range(NT) for kbi in range(4 * qi + 4) for c in range(2)]
        pend = []
        for st in steps:
            pend.append(st + (front(*st),))
            if len(pend) > LA:
                back(*pend.pop(0))
        while pend:
            back(*pend.pop(0))
        while thunks:
            thunks.pop(0)()
        kb.barrier()


def host_consts():
    jj = np.arange(128)[:, None]
    ii = np.arange(128)[None, :]
    same = (jj // 64) == (ii // 64)
    c = {}
    mprev = (jj >= ii).astype(np.float32)
    mcur = (jj <= ii).astype(np.float32)
    c["omask"] = np.concatenate([mprev, mcur, mprev, mcur], axis=1)
    qi = np.arange(512)[None, :]
    c["dmask"] = np.concatenate([((g * 128 + jj) <= qi).astype(np.float32) for g in range(4)], axis=1)
    gm = ((jj <= ii) & same).astype(np.float32)
    um = gm * (-1.0 / 16.0)
    lm = ((jj > ii) & same).astype(np.float32) * (-1.0 / 16.0)
    bd = same.astype(np.float32)
    c["gmasks"] = np.concatenate([gm, um, lm, bd], axis=1)
    return c


def setup_consts(kb, es, omask_d, dmask_d, gmasks_d):
    sc = kb.sc
    epsT = kb.sb(es, "eps_sb", [128, 1], F32)
    kb.eps_t = epsT
    kb.eps = Tile("eps", epsT[:])
    sc.op("pool", lambda e: e.memset(epsT[:], EPS), writes=[kb.eps])
    kb.one_t = kb.sb(es, "one_sb", [128, 1], F32)
    kb.one = Tile("one", kb.one_t[:])
    sc.op("pool", lambda e: e.memset(kb.one_t[:], 1.0), writes=[kb.one])
    kb.onesb_t = kb.sb(es, "onesb", [128, 128], BF16)
    kb.onesb = Tile("onesb", kb.onesb_t[:])
    sc.op("pool", lambda e: e.memset(kb.onesb_t[:], 1.0), writes=[kb.onesb])
    kb.omask_t = kb.sb(es, "omask_sb", [128, 512], BF16)
    kb.omask = Tile("omask", kb.omask_t[:])
    sc.op("pool", lambda e: e.dma_start(out=kb.omask_t[:], in_=omask_d), writes=[kb.omask], dma="cst")
    kb.dmask_t = kb.sb(es, "dmask_sb", [128, 4, 512], BF16)
    kb.dmask = Tile("dmask", kb.dmask_t[:])
    sc.op("pool", lambda e: e.dma_start(out=kb.dmask_t[:], in_=dmask_d.rearrange("p (g q) -> p g q", g=4)), writes=[kb.dmask], dma="cst")
    gmT = kb.sb(es, "gmasks_sb", [128, 4, 128], F32)
    kb.gmasks = Tile("gmasks", gmT[:])
    sc.op("sp", lambda e: e.dma_start(out=gmT[:], in_=gmasks_d.rearrange("p (g q) -> p g q", g=4)), writes=[kb.gmasks], dma="cst2")
    kb.gmask_t = gmT[:, 0]
    kb.umask_t = gmT[:, 1]
    kb.lmask_t = gmT[:, 2]
    kb.bd64_t = gmT[:, 3]
    kb.bd64r_t = kb.sb(es, "bd64r", [128, 128], F32)
    sc.op("dve", lambda e: e.tensor_copy(r32(kb.bd64r_t[:]), gmT[:, 3]), reads=[kb.gmasks], writes=[kb.gmasks])


DEPTH = 4
STAGE_MIX = False
S_FULL = 4096
NVEC = 780
VOFF = {
    "nm": lambda L: L * 192, "nf": lambda L: L * 192 + 8, "cw": lambda L: L * 192 + 16, "cb": lambda L: L * 192 + 148,
    "eg": lambda Le: 768 + Le * 4, "oq": lambda Lo: 776 + Lo * 2,
}


def build_program(S=S_FULL, depth=DEPTH):
    nc = bass.Bass("TRN2", target_bir_lowering=False)
    es = ExitStack()
    kb = KB(nc, es, S)
    NT = kb.NT
    xT = kb.din("xT", [D, S])
    vecs = kb.din("vecs", [128, NVEC])
    lvec = kb.din("lvec", [2, 128, 4, 64])
    wa2 = kb.din("wa2", [2, 16, 256])
    ba = kb.din("ba", [2, 1, 256])
    omask_d = kb.din("omask", [128, 512])
    dmask_d = kb.din("dmask", [128, 2048])
    gmasks_d = kb.din("gmasks", [128, 512])
    ev_w_in = kb.din("ev_w_in", [2, D, EVEN_IN])
    ev_w_out = kb.din("ev_w_out", [2, D, D])
    od_w_in = kb.din("od_w_in", [2, D, ODD_IN])
    od_w_out = kb.din("od_w_out", [2, 512, D])
    w_up = kb.din("ffn_w_up", [4, D, 2 * DFF])
    w_dn = kb.din("ffn_w_down", [4, DFF, D])
    yT = kb.dout("yT", [D, S])
    wup_s = kb.dscr("wup_s", [4, NJ, 128, 2, 8, 128], BF16)
    wdn_s = kb.dscr("wdn_s", [4, 8, 128, NJ, 128], BF16)
    qk_s = kb.dscr("qk_s", [3, 2, 4, 128, S], BF16)
    v_s = kb.dscr("v_s", [3, S, 512], BF16)
    od = kb.dscr("od_s", [1024, S], BF16)
    sd = {}
    kb.sd_t = {}
    for nm, shp, dt in (("gq", [256, S], BF16), ("gk", [256, S], BF16), ("gke", [S, 256], BF16), ("gv", [S, 512], BF16),
                        ("gg", [512, S], BF16), ("gdec", [256, S // 64], F32), ("dq", [512, S], BF16), ("dk", [512, S], BF16),
                        ("dv", [S, 512], BF16)):
        sd[nm] = kb.dscr("s_" + nm, shp, dt)
        kb.sd_t[nm] = Tile("sd_" + nm, None)
    win_s = [kb.dscr("win_s%d" % L, [128, 8, EVEN_IN if L % 2 == 0 else ODD_IN], BF16) for L in range(4)]
    wout_s = [kb.dscr("wout_s%d" % L, [128, 8 if L % 2 == 0 else 4, D], BF16) for L in range(4)]
    kb.VOFF = VOFF
    kb.wt = {}
    for L in range(4):
        kb.wt[("up", L)] = [Tile("tup%d_%d" % (L, j), None) for j in range(NJ)]
        kb.wt[("dn", L)] = [Tile("tdn%d_%d" % (L, d), None) for d in range(8)]
    vecT = kb.sb(es, "vecs_sb", [128, NVEC], F32)
    vec = Tile("vec", vecT[:])
    kb.sc.op("sp", lambda e: e.dma_start(out=vecT[:], in_=vecs), writes=[vec], dma="vec")
    setup_consts(kb, es, omask_d, dmask_d, gmasks_d)
    xin_t = [Tile("xin%d" % i, None) for i in range(NT)]
    y_t = [Tile("y%d" % i, None) for i in range(NT)]
    kb.od_t = [Tile("od%d" % i, None) for i in range(NT)]
    kb.qk_t = [[[Tile("qk", None) for _ in range(4)] for _ in range(2)] for _ in range(3)]
    kb.v_t = [Tile("v", None) for _ in range(3)]

    def make_hook(th):
        per = -(-len(th) // NT)

        def hook(tt):
            for f in th[tt * per:(tt + 1) * per]:
                f()
        return hook

    def mix_thunks(L):
        if L % 2 == 0:
            th = prep_mix_weights(kb, ("win", L), ev_w_in[L // 2].rearrange("(kc p) n -> p kc n", p=128), win_s[L], EVEN_IN)
            th += prep_mix_weights(kb, ("wout", L), ev_w_out[L // 2].rearrange("(kc p) n -> p kc n", p=128), wout_s[L], D)
        else:
            th = prep_mix_weights(kb, ("win", L), od_w_in[L // 2].rearrange("(kc p) n -> p kc n", p=128), win_s[L], ODD_IN)
            th += prep_mix_weights(kb, ("wout", L), od_w_out[L // 2].rearrange("(kc p) n -> p kc n", p=128), wout_s[L], D)
        return th

    kb.stage_mix = STAGE_MIX
    if STAGE_MIX:
        for f_ in mix_thunks(0):
            f_()
    for L in range(depth):
        x_src, xs_t = (xT, xin_t) if L == 0 else (yT, y_t)
        hook = make_hook(prep_ffn_weights(kb, L, w_up, w_dn, wup_s, wdn_s))
        th2 = mix_thunks(L + 1) if (L + 1 < depth and STAGE_MIX) else []
        v3 = lambda w: w.rearrange("(kc p) n -> p kc n", p=128)
        if STAGE_MIX:
            wi, wo = win_s[L], wout_s[L]
        elif L % 2 == 0:
            wi, wo = v3(ev_w_in[L // 2]), v3(ev_w_out[L // 2])
        else:
            wi, wo = v3(od_w_in[L // 2]), v3(od_w_out[L // 2])
        if L % 2 == 0:
            Le = L // 2
            phase_even_a(kb, L, Le, x_src, xs_t, wi, wa2, ba, sd, vec, hook=hook)
            phase_even_gla(kb, Le, sd, od, vec)
            phase_even_diff(kb, L, Le, sd, od, vec, lvec, thunks=th2)
            mix = (od, wo, 8)
        else:
            Lo = L // 2
            phase_odd_a(kb, L, Lo, x_src, xs_t, wi, qk_s, v_s, vec, hook=hook)
            phase_odd_b(kb, qk_s, v_s, od, thunks=th2)
            mix = (od[0:512], wo, 4)
        phase_ffn(kb, L, x_src, xs_t, yT, y_t, wup_s, wdn_s, vec, mix=mix)
    kb.sc.emit(final_waits=[(k, c) for k, c in kb.sc.dma_cnt.items() if k.startswith("st_")])
    es.close()
    return nc, kb


def host_vecs(inp):
    v = np.zeros((128, NVEC), np.float32)
    col = lambda a: np.asarray(a, np.float32).reshape(-1, 128).T
    for L in range(4):
        v[:, VOFF["nm"](L):VOFF["nm"](L) + 8] = col(inp["norm_mix"][L])
        v[:, VOFF["nf"](L):VOFF["nf"](L) + 8] = col(inp["norm_ffn"][L])
        cw = np.asarray(inp["ffn_conv_w"][L], np.float32).reshape(3, 2 * DFF)
        for tap in range(3):
            v[:, VOFF["cw"](L) + tap * 44:VOFF["cw"](L) + (tap + 1) * 44] = col(cw[tap])
        v[:, VOFF["cb"](L):VOFF["cb"](L) + 44] = col(inp["ffn_conv_b"][L])
    for e in range(2):
        b = VOFF["eg"](e)
        v[:, b] = inp["ev_gla_gain"][e]
        v[:, b + 1] = np.tile(np.asarray(inp["ev_dq_gain"][e], np.float32), 2)
        v[:, b + 2] = np.tile(np.asarray(inp["ev_dk_gain"][e], np.float32), 2)
        v[:, b + 3] = inp["ev_diff_gain"][e]
    for o in range(2):
        b = VOFF["oq"](o)
        v[:, b] = inp["od_q_gain"][o]
        v[:, b + 1] = inp["od_k_gain"][o]
    return v


_CACHE = {}


def kernel(**inputs):
    inp = {k: np.asarray(v) for k, v in inputs.items()}
    x = inp["x"].astype(np.float32, copy=False)
    B = x.shape[0]
    if "nc" not in _CACHE:
        _CACHE["nc"] = build_program()[0]
    nc = _CACHE["nc"]
    hc = host_consts()
    lv = np.stack([np.stack([inp["ev_lq1"][e], inp["ev_lk1"][e], inp["ev_lq2"][e], inp["ev_lk2"][e]], 0) for e in range(2)], 0)
    lvec = np.ascontiguousarray(np.broadcast_to(lv[:, None].astype(np.float32), (2, 128, 4, 64)))
    shared = {
        "vecs": host_vecs(inp), "lvec": lvec,
        "wa2": np.ascontiguousarray(inp["ev_w_a2"], np.float32),
        "ba": np.ascontiguousarray(inp["ev_b_a"], np.float32).reshape(2, 1, 256),
        "ev_w_in": np.ascontiguousarray(inp["ev_w_in"], np.float32), "ev_w_out": np.ascontiguousarray(inp["ev_w_out"], np.float32),
        "od_w_in": np.ascontiguousarray(inp["od_w_in"], np.float32), "od_w_out": np.ascontiguousarray(inp["od_w_out"], np.float32),
        "ffn_w_up": np.ascontiguousarray(inp["ffn_w_up"], np.float32), "ffn_w_down": np.ascontiguousarray(inp["ffn_w_down"], np.float32),
        **hc,
    }
    in_maps = [dict(shared, xT=np.ascontiguousarray(x[b].T)) for b in range(B)]
    res = run_bass_kernel_spmd(nc, in_maps, core_ids=list(range(B)))
    out = np.stack([np.asarray(res.results[b]["yT"], np.float32).T for b in range(B)], 0)
    return np.ascontiguousarray(out, dtype=np.float32)
```

```python
import numpy as np
from contextlib import ExitStack
import concourse.bass as bass
import concourse.mybir as mybir
from concourse.bass_utils import run_bass_kernel_spmd
F32 = mybir.dt.float32
BF16 = mybir.dt.bfloat16
AF = mybir.ActivationFunctionType
ALU = mybir.AluOpType
AX = mybir.AxisListType

SEM_LIMIT = 30000


class Tile:
    __slots__ = ("name", "ap", "w", "r")

    def __init__(self, name, ap):
        self.name = name
        self.ap = ap
        self.w = None
        self.r = []

    def __getitem__(self, k):
        return self.ap[k]


class Op:
    __slots__ = ("eng", "fn", "waits", "sig", "ms", "semkey", "dma", "idx")


class Sched:
    ENGS = ("pe", "act", "dve", "pool", "sp")

    def __init__(self, nc, es, sync_same_engine=True):
        self.nc = nc
        self.es = es
        self.ops = {e: [] for e in self.ENGS}
        self.nsig = {e: 0 for e in self.ENGS}
        self.dma_cnt = {}
        self.known = {e: {} for e in self.ENGS}
        self.sync_same = sync_same_engine
        self.sems = {}
        self.n_ops = 0
        self.pending = {e: [] for e in self.ENGS}

    def op(self, eng, fn, reads=(), writes=(), dma=None):
        o = Op()
        o.eng = eng
        o.fn = fn
        o.sig = False
        o.ms = None
        o.dma = dma
        o.idx = self.n_ops
        self.n_ops += 1
        if self.pending[eng]:
            reads = list(reads) + self.pending[eng]
            self.pending[eng] = []
        deps = []
        for t in reads:
            if t.w is not None:
                deps.append(t.w)
        for t in writes:
            if t.w is not None:
                deps.append(t.w)
            deps.extend(t.r)
        cdeps = {}
        dwaits = {}
        for p in deps:
            if p is o:
                continue
            if p.dma is not None:
                val = self.dma_cnt[p.dma] * 16
                if dwaits.get(p.dma, 0) < val:
                    dwaits[p.dma] = val
            else:
                if p.eng == eng and (eng == "pe" or not self.sync_same):
                    continue
                p.sig = True
                q = cdeps.get(p.eng)
                if q is None or q.idx < p.idx:
                    cdeps[p.eng] = p
        o.waits = (list(cdeps.values()), list(dwaits.items()))
        if dma is not None:
            self.dma_cnt[dma] = self.dma_cnt.get(dma, 0) + 1
        for t in reads:
            t.r.append(o)
        for t in writes:
            t.w = o
            t.r = []
        self.ops[eng].append(o)
        return o

    def _sem(self, key):
        s = self.sems.get(key)
        if s is None:
            s = self.es.enter_context(self.nc.semaphore("s_%s_%s" % (key[0], str(key[1]).replace(" ", "_"))))
            self.sems[key] = s
        return s

    def emit(self, final_waits=()):
        nc = self.nc
        block = self.es.enter_context(nc.Block())
        for e in self.ENGS:
            n = 0
            for o in self.ops[e]:
                if o.dma is None and o.sig:
                    n += 1
                    o.ms = n
        for e in self.ENGS:
            kn = {}
            for o in self.ops[e]:
                cd, dw = o.waits
                w = {}
                for p in cd:
                    key = (p.eng, (p.ms - 1) // SEM_LIMIT)
                    val = (p.ms - 1) % SEM_LIMIT + 1
                    if w.get(key, 0) < val:
                        w[key] = val
                for k, val in dw:
                    key = ("dma", k)
                    if w.get(key, 0) < val:
                        w[key] = val
                res = []
                for key, val in w.items():
                    if kn.get(key, 0) >= val:
                        continue
                    kn[key] = val
                    res.append((key, val))
                    self._sem(key)
                o.waits = res
                if o.dma is not None:
                    self._sem(("dma", o.dma))
                elif o.sig:
                    self._sem((o.eng, (o.ms - 1) // SEM_LIMIT))
        for key, cnt in final_waits:
            self._sem(("dma", key))

        def run(ename):
            def body(eng):
                for o in self.ops[ename]:
                    for key, val in o.waits:
                        eng.wait_ge(self.sems[key], val)
                    ins = o.fn(eng)
                    if o.dma is not None:
                        ins.then_inc(self.sems[("dma", o.dma)], 16)
                    elif o.sig:
                        ins.then_inc(self.sems[(o.eng, (o.ms - 1) // SEM_LIMIT)], 1)
                if ename == "sp":
                    for key, cnt in final_waits:
                        eng.wait_ge(self.sems[("dma", key)], cnt * 16)
            return body

        if self.ops["sp"] or final_waits:
            block.sync(run("sp"))
        if self.ops["pe"]:
            block.tensor(run("pe"))
        if self.ops["act"]:
            block.scalar(run("act"))
        if self.ops["dve"]:
            block.vector(run("dve"))
        if self.ops["pool"]:
            block.gpsimd(run("pool"))


    def barrier(self):
        ts = []
        for e in ("pe", "act", "dve", "pool"):
            if self.ops[e]:
                t = Tile("bar_" + e, None)
                t.w = self.ops[e][-1]
                ts.append(t)
        for key in list(self.dma_cnt.keys()):
            t = Tile("bard_" + key, None)
            o = Op()
            o.eng = "sp"; o.dma = key; o.sig = False; o.ms = None; o.idx = -1
            t.w = o
            ts.append(t)
        for e in self.ENGS:
            self.pending[e] = list(ts)


class Ring:
    def __init__(self, tiles):
        self.tiles = tiles
        self.i = 0

    def next(self):
        t = self.tiles[self.i % len(self.tiles)]
        self.i += 1
        return t


D = 1024
DFF = 2816
NJ = DFF // 128
EPS = 1e-6
TT = 512
USE_F32R = True
F32R = mybir.dt.float32r


def r32(ap):
    return ap.bitcast(F32R) if USE_F32R else ap


class KB:
    def __init__(self, nc, es, S):
        self.nc = nc
        self.es = es
        self.S = S
        self.NT = S // TT
        self.sc = Sched(nc, es)
        self.dram = {}
        self.psum = []
        for i in range(8):
            t = es.enter_context(nc.psum_tensor("psb%d" % i, [128, 512], F32))
            self.psum.append(t)
        self.ones32_t = es.enter_context(nc.sbuf_tensor("ones32", [128, 128], F32))
        self.ones32 = Tile("ones32", self.ones32_t[:])
        self.ones32raw_t = es.enter_context(nc.sbuf_tensor("ones32raw", [128, 128], F32))
        raw = Tile("ones32raw", self.ones32raw_t[:])
        self.sc.op("pool", lambda e: e.memset(self.ones32raw_t[:], 1.0), writes=[raw])
        self.sc.op("dve", lambda e: e.tensor_copy(r32(self.ones32_t[:]), self.ones32raw_t[:]), reads=[raw], writes=[self.ones32])

    def din(self, name, shape, dt=F32):
        t = self.nc.dram_tensor(name, list(shape), dt, kind="ExternalInput")
        self.dram[name] = t
        return t.ap()

    def dout(self, name, shape, dt=F32):
        t = self.nc.dram_tensor(name, list(shape), dt, kind="ExternalOutput")
        self.dram[name] = t
        return t.ap()

    def dscr(self, name, shape, dt):
        t = self.nc.dram_tensor(name, list(shape), dt, kind="Internal")
        self.dram[name] = t
        return t.ap()

    def sb(self, pes, name, shape, dt):
        self._uid = getattr(self, "_uid", 0) + 1
        return pes.enter_context(self.nc.sbuf_tensor("%s_u%d" % (name, self._uid), list(shape), dt))

    def ptiles(self, tag):
        return [Tile("%s_ps%d" % (tag, i), self.psum[i][:]) for i in range(8)]

    def barrier(self):
        self.sc.barrier()


def prep_ffn_weights(kb, L, w_up, w_down, wup_s, wdn_s):
    sc = kb.sc
    src_up = w_up[L].rearrange("(kc p) (gv j c) -> j p gv kc c", p=128, gv=2, j=NJ)
    src_dn = w_down[L].rearrange("(j p) (d c) -> d p j c", p=128, d=8)
    t_up = kb.wt[("up", L)]
    t_dn = kb.wt[("dn", L)]
    th = []
    for j in range(NJ):
        th.append(lambda j=j: sc.op("pool", lambda e: e.dma_start(out=wup_s[L, j], in_=src_up[j]),
                                    writes=[t_up[j]], dma="wprep%d" % L))
    for d in range(8):
        th.append(lambda d=d: sc.op("pool", lambda e: e.dma_start(out=wdn_s[L, d], in_=src_dn[d]),
                                    writes=[t_dn[d]], dma="wprep%d" % L))
    return th


def phase_ffn(kb, L, x_src, xs_t, x_dst, xd_t, wup_s, wdn_s, vec, mix=None, hook=None):
    nc, sc, S, NT = kb.nc, kb.sc, kb.S, kb.NT
    with ExitStack() as pes:
        xT = kb.sb(pes, "f_x", [128, 2, 8, TT], F32)
        hT = kb.sb(pes, "f_h", [128, 2, 8, TT], BF16)
        sqT = kb.sb(pes, "f_sq", [128, 8, TT], F32)
        lnT = kb.sb(pes, "f_ln", [128, TT], F32)
        rsT = kb.sb(pes, "f_rs", [128, TT], F32)
        aT = kb.sb(pes, "f_a", [128, NJ, TT], BF16)
        gT = kb.sb(pes, "f_g", [128, 3, TT], F32)
        vT = kb.sb(pes, "f_v", [128, 3, TT], F32)
        wuT = kb.sb(pes, "f_wu", [128, 4, 2, 8, 128], BF16)
        wdT = kb.sb(pes, "f_wd", [128, 3, NJ, 128], BF16)
        hlT = kb.sb(pes, "f_hl", [128, 2, 2 * NJ, 2], F32)
        x_r = Ring([Tile("x%d" % i, xT[:, i]) for i in range(2)])
        h_r = Ring([Tile("h%d" % i, hT[:, i]) for i in range(2)])
        sq_r = Ring([Tile("sq%d" % i, sqT[:, i]) for i in range(8)])
        ln_t = Tile("ln", lnT[:])
        rs_t = Tile("rs", rsT[:])
        a_t = [Tile("a%d" % j, aT[:, j]) for j in range(NJ)]
        g_r = Ring([Tile("g%d" % i, gT[:, i]) for i in range(3)])
        v_r = Ring([Tile("v%d" % i, vT[:, i]) for i in range(3)])
        wu_r = Ring([Tile("wu%d" % i, wuT[:, i]) for i in range(4)])
        wd_r = Ring([Tile("wd%d" % i, wdT[:, i]) for i in range(3)])
        hl_t = [[Tile("hl%d_%d" % (pp_, q), hlT[:, pp_, q]) for q in range(2 * NJ)] for pp_ in range(2)]
        ps = kb.ptiles("f")
        ss_ps = ps[0]
        gp_r = Ring([ps[1], ps[2]])
        vp_r = Ring([ps[3], ps[4]])
        y_r = Ring([ps[5], ps[6], ps[7]])
        if mix is not None:
            oT_d, wout_s, KC = mix
            oT = kb.sb(pes, "f_o", [128, 2, KC, TT], BF16)
            woT = kb.sb(pes, "f_wo", [128, KC, D], BF16)
            o_r = Ring([Tile("o%d" % i, oT[:, i]) for i in range(2)])
            wo_bt = load_cast_weight(kb, woT, "wo", wout_s, D)
            wo_tiles = lambda d: [wo_bt[(d * 128) // 512]]
        xs_v = x_src.rearrange("(c p) t -> p c t", p=128)
        xd_v = x_dst.rearrange("(c p) t -> p c t", p=128)
        t_up = kb.wt[("up", L)]
        t_dn = kb.wt[("dn", L)]
        V = kb.VOFF
        vt = vec

        def load_tile(tt):
            xt = x_r.next()
            sc.op("sp", lambda e, xt=xt, tt=tt: e.dma_start(out=xt.ap, in_=xs_v[:, :, tt * TT:(tt + 1) * TT]),
                  reads=[xs_t[tt]], writes=[xt], dma=xt.name)
            ot = None
            if mix is not None:
                ot = o_r.next()
                ov = oT_d.rearrange("(c p) t -> p c t", p=128)
                sc.op("sp", lambda e, ot=ot, tt=tt: e.dma_start(out=ot.ap, in_=ov[:, :, tt * TT:(tt + 1) * TT]),
                      reads=[kb.od_t[tt]], writes=[ot], dma=ot.name)
            return xt, ot

        def preA(tt, xt, ot):
            if hook is not None:
                hook(tt)
            if mix is not None:
                for d in range(8):
                    yp = y_r.next()
                    for kc in range(KC):
                        sc.op("pe", lambda e, yp=yp, kc=kc, d=d: e.matmul(
                            yp.ap, woT[:, kc, d * 128:(d + 1) * 128], ot.ap[:, kc], start=(kc == 0), stop=(kc == KC - 1)),
                            reads=wo_tiles(d) + [ot], writes=[yp])
                    sc.op("dve", lambda e, yp=yp, d=d: e.tensor_tensor(
                        xt.ap[:, d], yp.ap, xt.ap[:, d], ALU.add), reads=[yp, xt], writes=[xt])
            sqs = []
            for c in range(8):
                sq = sq_r.next()
                sc.op("act", lambda e, sq=sq, c=c: e.activation(r32(sq.ap), xt.ap[:, c], AF.Square),
                      reads=[xt], writes=[sq])
                sqs.append(sq)
            return sqs

        def preB(tt, xt, sqs):
            for c in range(8):
                sq = sqs[c]
                sc.op("pe", lambda e, sq=sq, c=c: e.matmul(ss_ps.ap, r32(kb.ones32_t[:]), r32(sq.ap), start=(c == 0), stop=(c == 7)),
                      reads=[sq, kb.ones32], writes=[ss_ps])
            sc.op("act", lambda e: e.activation(lnT[:], ss_ps.ap, AF.Ln, bias=kb.eps_t[:], scale=1.0 / D),
                  reads=[ss_ps, kb.eps], writes=[ln_t])
            sc.op("act", lambda e: e.activation(rsT[:], lnT[:], AF.Exp, scale=-0.5), reads=[ln_t], writes=[rs_t])
            ht = h_r.next()
            for c in range(8):
                sc.op("dve", lambda e, c=c: e.scalar_tensor_tensor(
                    ht.ap[:, c], xt.ap[:, c], vt.ap[:, V["nf"](L) + c:V["nf"](L) + c + 1], rsT[:], ALU.mult, ALU.mult),
                    reads=[xt, rs_t, vt], writes=[ht])
            return ht

        def up(tt, xt, ht):
            for j in range(NJ):
                wu = wu_r.next()
                sc.op("sp", lambda e, wu=wu, j=j: e.dma_start(out=wu.ap, in_=wup_s[L, j]), reads=[t_up[j]],
                      writes=[wu], dma=wu.name)
                outs = []
                for gv in range(2):
                    pp = (gp_r if gv == 0 else vp_r).next()
                    for kc in range(8):
                        sc.op("pe", lambda e, pp=pp, wu=wu, gv=gv, kc=kc, ht=ht: e.matmul(
                            pp.ap, wu.ap[:, gv, kc, :], ht.ap[:, kc], start=(kc == 0), stop=(kc == 7)),
                            reads=[wu, ht], writes=[pp])
                    q = gv * NJ + j
                    dst = (g_r if gv == 0 else v_r).next()
                    cw = lambda tap, q=q: vt.ap[:, V["cw"](L) + tap * 2 * NJ + q:V["cw"](L) + tap * 2 * NJ + q + 1]
                    cb = vt.ap[:, V["cb"](L) + q:V["cb"](L) + q + 1]
                    sc.op("act", lambda e, dst=dst, pp=pp, cw=cw, cb=cb: e.activation(
                        dst.ap, pp.ap, AF.Identity, bias=cb, scale=cw(2)), reads=[pp, vt], writes=[dst])
                    sc.op("dve", lambda e, dst=dst, pp=pp, cw=cw: e.scalar_tensor_tensor(
                        dst.ap[:, 1:TT], pp.ap[:, 0:TT - 1], cw(1), dst.ap[:, 1:TT], ALU.mult, ALU.add),
                        reads=[pp, dst, vt], writes=[dst])
                    sc.op("dve", lambda e, dst=dst, pp=pp, cw=cw: e.scalar_tensor_tensor(
                        dst.ap[:, 2:TT], pp.ap[:, 0:TT - 2], cw(0), dst.ap[:, 2:TT], ALU.mult, ALU.add),
                        reads=[pp, dst, vt], writes=[dst])
                    hl = hl_t[tt % 2][q]
                    hln = hl_t[(tt + 1) % 2][q]
                    if tt + 1 < NT:
                        sc.op("act", lambda e, hln=hln, pp=pp: e.activation(hln.ap, pp.ap[:, TT - 2:TT], AF.Copy),
                              reads=[pp], writes=[hln])
                    if tt > 0:
                        sc.op("dve", lambda e, dst=dst, hl=hl, cw=cw: e.scalar_tensor_tensor(
                            dst.ap[:, 0:2], hl.ap[:, 0:2], cw(0), dst.ap[:, 0:2], ALU.mult, ALU.add),
                            reads=[hl, dst, vt], writes=[dst])
                        sc.op("dve", lambda e, dst=dst, hl=hl, cw=cw: e.scalar_tensor_tensor(
                            dst.ap[:, 0:1], hl.ap[:, 1:2], cw(1), dst.ap[:, 0:1], ALU.mult, ALU.add),
                            reads=[hl, dst, vt], writes=[dst])
                    outs.append(dst)
                gd, vd = outs
                sc.op("act", lambda e, gd=gd: e.activation(gd.ap, gd.ap, AF.Silu), reads=[gd], writes=[gd])
                sc.op("pool", lambda e, gd=gd, vd=vd, j=j: e.tensor_tensor(a_t[j].ap, gd.ap, vd.ap, ALU.mult),
                      reads=[gd, vd], writes=[a_t[j]])

        def down(tt, xt, d0, d1):
            for d in range(d0, d1):
                wd = wd_r.next()
                sc.op("sp", lambda e, wd=wd, d=d: e.dma_start(out=wd.ap, in_=wdn_s[L, d]), reads=[t_dn[d]],
                      writes=[wd], dma=wd.name)
                yp = y_r.next()
                for j in range(NJ):
                    sc.op("pe", lambda e, yp=yp, wd=wd, j=j: e.matmul(
                        yp.ap, wd.ap[:, j, :], a_t[j].ap, start=(j == 0), stop=(j == NJ - 1)),
                        reads=[wd, a_t[j]], writes=[yp])
                sc.op("dve", lambda e, yp=yp, d=d, xt=xt: e.tensor_tensor(
                    xt.ap[:, d], yp.ap, xt.ap[:, d], ALU.add), reads=[yp, xt], writes=[xt])
            if d1 < 8:
                return
            sc.op("act", lambda e, xt=xt, tt=tt: e.dma_start(out=xd_v[:, :, tt * TT:(tt + 1) * TT], in_=xt.ap),
                  reads=[xt], writes=[xd_t[tt]], dma="st_" + xt.name)

        tiles = {}
        tiles[0] = load_tile(0)
        hts = {}
        hts[0] = preB(0, tiles[0][0], preA(0, *tiles[0]))
        for tt in range(NT):
            up(tt, tiles[tt][0], hts[tt])
            if tt + 1 < NT:
                tiles[tt + 1] = load_tile(tt + 1)
                sqs = preA(tt + 1, *tiles[tt + 1])
                down(tt, tiles[tt][0], 0, 4)
                hts[tt + 1] = preB(tt + 1, tiles[tt + 1][0], sqs)
                down(tt, tiles[tt][0], 4, 8)
            else:
                down(tt, tiles[tt][0], 0, 8)
        kb.barrier()


ODD_IN = 4608
DILS = (1, 4, 16)


def load_cast_weight(kb, dst_t, name, src_v, ncols, step=512):
    tiles = []
    for c0 in range(0, ncols, step):
        c1 = min(ncols, c0 + step)
        t = Tile("%s_b%d" % (name, c0 // step), None)
        kb.sc.op("pool", lambda e, c0=c0, c1=c1: e.dma_start(out=dst_t[:, :, c0:c1], in_=src_v[:, :, c0:c1]),
                 writes=[t], dma="w_" + name)
        tiles.append(t)
    return tiles


def wblk(tiles, c0, c1, step=512):
    return tiles[c0 // step:(c1 - 1) // step + 1]


def norm_tile(kb, xt, ss_ps, sq_r, lnT, ln_t, rsT, rs_t):
    sc = kb.sc
    for c in range(8):
        sq = sq_r.next()
        sc.op("act", lambda e, sq=sq, c=c: e.activation(r32(sq.ap), xt.ap[:, c], AF.Square), reads=[xt], writes=[sq])
        sc.op("pe", lambda e, sq=sq, c=c: e.matmul(ss_ps.ap, r32(kb.ones32_t[:]), r32(sq.ap), start=(c == 0), stop=(c == 7)),
              reads=[sq, kb.ones32], writes=[ss_ps])
    sc.op("act", lambda e: e.activation(lnT[:], ss_ps.ap, AF.Ln, bias=kb.eps_t[:], scale=1.0 / D),
          reads=[ss_ps, kb.eps], writes=[ln_t])
    sc.op("act", lambda e: e.activation(rsT[:], lnT[:], AF.Exp, scale=-0.5), reads=[ln_t], writes=[rs_t])


def phase_odd_a(kb, L, Lo, x_src, xs_t, w_in, qk_s, v_s, vec, hook=None):
    nc, sc, S, NT = kb.nc, kb.sc, kb.S, kb.NT
    V = kb.VOFF
    with ExitStack() as pes:
        wT = kb.sb(pes, "oa_w", [128, 8, ODD_IN], BF16)
        w_bt = load_cast_weight(kb, wT, "oa_w", w_in[Lo].rearrange("(kc p) n -> p kc n", p=128), ODD_IN)
        xT = kb.sb(pes, "oa_x", [128, 2, 8, TT], F32)
        h0T = kb.sb(pes, "oa_h0", [128, 2, 8, TT], BF16)
        h1T = kb.sb(pes, "oa_h1", [128, 2, 8, TT], BF16)
        h2T = kb.sb(pes, "oa_h2", [128, 8, 4 * TT], BF16)
        sqT = kb.sb(pes, "oa_sq", [128, 2, TT], F32)
        lnT = kb.sb(pes, "oa_ln", [128, TT], F32)
        rsT = kb.sb(pes, "oa_rs", [128, TT], F32)
        sq2T = kb.sb(pes, "oa_sq2", [128, 3, TT], F32)
        ln2T = kb.sb(pes, "oa_ln2", [128, 2, TT], F32)
        rs2T = kb.sb(pes, "oa_rs2", [128, 2, TT], F32)
        qoT = kb.sb(pes, "oa_qo", [128, 3, TT], BF16)
        voT = kb.sb(pes, "oa_vo", [128, 2, TT], BF16)
        x_r = Ring([Tile("x%d" % i, xT[:, i]) for i in range(2)])
        h0_r = Ring([[Tile("h0%d_%d" % (i, c), h0T[:, i, c]) for c in range(8)] for i in range(2)])
        h1_r = Ring([[Tile("h1%d_%d" % (i, c), h1T[:, i, c]) for c in range(8)] for i in range(2)])
        h2_t = [Tile("h2_%d" % c, h2T[:, c]) for c in range(8)]
        sq_r = Ring([Tile("sq%d" % i, sqT[:, i]) for i in range(2)])
        ln_t = Tile("ln", lnT[:])
        rs_t = Tile("rs", rsT[:])
        sq2_r = Ring([Tile("sq2%d" % i, sq2T[:, i]) for i in range(3)])
        ln2_r = Ring([Tile("ln2%d" % i, ln2T[:, i]) for i in range(2)])
        rs2_r = Ring([Tile("rs2%d" % i, rs2T[:, i]) for i in range(2)])
        qo_r = Ring([Tile("qo%d" % i, qoT[:, i]) for i in range(3)])
        vo_r = Ring([Tile("vo%d" % i, voT[:, i]) for i in range(2)])
        ps = kb.ptiles("oa")
        ss_ps = ps[0]
        z_r = Ring([ps[1], ps[2], ps[3]])
        s2_r = Ring([ps[4], ps[5]])
        vp_r = Ring([ps[6], ps[7]])
        xs_v = x_src.rearrange("(c p) t -> p c t", p=128)

        def load_tile(tt):
            xt = x_r.next()
            sc.op("sp", lambda e, xt=xt, tt=tt: e.dma_start(out=xt.ap, in_=xs_v[:, :, tt * TT:(tt + 1) * TT]),
                  reads=[xs_t[tt]], writes=[xt], dma=xt.name)
            return xt

        def do_proj(g, hv, hv_t, pos0, extra=None):
            def finish(zp, sq, qk, hh):
                gcol = V["oq"](Lo) + qk
                sp2 = s2_r.next()
                sc.op("pe", lambda e: e.matmul(sp2.ap, r32(kb.ones32_t[:]), r32(sq.ap), start=True, stop=True),
                      reads=[sq, kb.ones32], writes=[sp2])
                ln = ln2_r.next()
                sc.op("act", lambda e: e.activation(ln.ap, sp2.ap, AF.Ln, bias=kb.eps_t[:], scale=1.0 / 128),
                      reads=[sp2, kb.eps], writes=[ln])
                rs = rs2_r.next()
                sc.op("act", lambda e: e.activation(rs.ap, ln.ap, AF.Exp, scale=-0.5), reads=[ln], writes=[rs])
                qo = qo_r.next()
                sc.op("dve", lambda e: e.scalar_tensor_tensor(
                    qo.ap, zp.ap, vec.ap[:, gcol:gcol + 1], rs.ap, ALU.mult, ALU.mult),
                    reads=[zp, rs, vec], writes=[qo])
                sc.op("sp", lambda e: e.dma_start(
                    out=qk_s[g, qk, hh, :, pos0:pos0 + TT], in_=qo.ap), reads=[qo], writes=[kb.qk_t[g][qk][hh]], dma="st_" + qo.name)

            def vsub(sub):
                vp = vp_r.next()
                for kc in range(8):
                    sc.op("pe", lambda e, kc=kc: e.matmul(
                        vp.ap, hv[:, kc, sub * 128:(sub + 1) * 128], wT[:, kc, vcol:vcol + 512], start=(kc == 0), stop=(kc == 7)),
                        reads=wblk(w_bt, vcol, vcol + 512) + [hv_t[kc]], writes=[vp])
                vo = vo_r.next()
                sc.op("act", lambda e: e.activation(vo.ap, vp.ap, AF.Copy), reads=[vp], writes=[vo])
                sc.op("sp", lambda e: e.dma_start(
                    out=v_s[g, pos0 + sub * 128:pos0 + (sub + 1) * 128, :], in_=vo.ap), reads=[vo], writes=[kb.v_t[g]], dma="st_" + vo.name)

            vcol = ((g * 3 + 2) * 4) * 128
            pend = []
            for qk in range(2):
                for hh in range(4):
                    col = ((g * 3 + qk) * 4 + hh) * 128
                    zp = z_r.next()
                    for kc in range(8):
                        sc.op("pe", lambda e, zp=zp, kc=kc, col=col: e.matmul(
                            zp.ap, wT[:, kc, col:col + 128], hv[:, kc], start=(kc == 0), stop=(kc == 7)),
                            reads=wblk(w_bt, col, col + 128) + [hv_t[kc]], writes=[zp])
                    sq = sq2_r.next()
                    sc.op("act", lambda e, sq=sq, zp=zp: e.activation(r32(sq.ap), zp.ap, AF.Square), reads=[zp], writes=[sq])
                    pend.append((zp, sq, qk, hh))
                    if len(pend) > 1:
                        finish(*pend.pop(0))
                    if extra:
                        extra.pop(0)()
            vsub(0)
            finish(*pend.pop(0))
            for sub in range(1, 4):
                vsub(sub)

        def prep(tt, xt):
            if hook is not None:
                hook(tt)
            norm_tile(kb, xt, ss_ps, sq_r, lnT, ln_t, rsT, rs_t)
            h0 = h0_r.next()
            h1 = h1_r.next()
            s4 = tt % 4
            for c in range(8):
                gc = V["nm"](L) + c
                sc.op("dve", lambda e, c=c, gc=gc: e.scalar_tensor_tensor(
                    h0[c].ap, xt.ap[:, c], vec.ap[:, gc:gc + 1], rsT[:], ALU.mult, ALU.mult),
                    reads=[xt, rs_t, vec], writes=[h0[c]])
            for c in range(8):
                sc.op("pool", lambda e, c=c: e.tensor_copy(
                    h2T[:, c].rearrange("p (r i) -> p i r", r=16)[:, 32 * s4:32 * s4 + 32, :],
                    h0[c].ap.rearrange("p (i r) -> p i r", r=16)),
                    reads=[h0[c]], writes=[h2_t[c]])
            ex = []
            for c in range(8):
                gc = V["nm"](L) + c
                ex.append(lambda c=c, gc=gc: sc.op("dve", lambda e: e.scalar_tensor_tensor(
                    h1[c].ap.rearrange("p (r i) -> p i r", r=4), xt.ap[:, c].rearrange("p (i r) -> p i r", r=4),
                    vec.ap[:, gc:gc + 1], rsT[:].rearrange("p (i r) -> p i r", r=4), ALU.mult, ALU.mult),
                    reads=[xt, rs_t, vec], writes=[h1[c]]))
            return h0, h1, ex

        class _HV:
            def __init__(self, tiles):
                self.tiles = tiles

            def __getitem__(self, k):
                kc = k[1]
                ap = self.tiles[kc].ap
                return ap if len(k) == 2 else ap[:, k[2]]

        xts = {0: load_tile(0)}
        pr = {0: prep(0, xts[0])}
        for tt in range(NT):
            h0, h1, ex = pr[tt]
            if tt + 1 < NT:
                xts[tt + 1] = load_tile(tt + 1)
            do_proj(0, _HV(h0), h0, tt * TT, extra=ex)
            while ex:
                ex.pop(0)()
            if tt + 1 < NT and tt % 4 != 3:
                pr[tt + 1] = prep(tt + 1, xts[tt + 1])
            do_proj(1, _HV(h1), h1, tt * TT)
            if tt % 4 == 3:
                for s in range(4):
                    class _H2:
                        def __init__(self, s):
                            self.s = s

                        def __getitem__(self, k):
                            kc = k[1]
                            ap = h2T[:, kc, self.s * TT:(self.s + 1) * TT]
                            return ap if len(k) == 2 else ap[:, k[2]]
                    do_proj(2, _H2(s), h2_t, (tt // 4) * 4 * TT + s * TT)
                if tt + 1 < NT:
                    pr[tt + 1] = prep(tt + 1, xts[tt + 1])
        kb.barrier()


def phase_odd_b(kb, qk_s, v_s, od):
    nc, sc, S, NT = kb.nc, kb.sc, kb.S, kb.NT
    NB = S // 128
    with ExitStack() as pes:
        accO2 = kb.sb(pes, "ob_ao", [128, 2, S], F32)
        accD2 = kb.sb(pes, "ob_ad", [128, 2, S], F32)
        qT = kb.sb(pes, "ob_q", [128, 2, S], BF16)
        kT = kb.sb(pes, "ob_k", [128, 2, S], BF16)
        vT = kb.sb(pes, "ob_v", [128, 2, NB, 128], BF16)
        pT = kb.sb(pes, "ob_p", [128, 4, 512], BF16)
        rdT = kb.sb(pes, "ob_rd", [128, 2, 512], F32)
        ooT = kb.sb(pes, "ob_oo", [128, 2, 512], BF16)
        ao_t2 = [Tile("accO%d" % i, accO2[:, i]) for i in range(2)]
        ad_t2 = [Tile("accD%d" % i, accD2[:, i]) for i in range(2)]
        q_r = Ring([Tile("q%d" % i, qT[:, i]) for i in range(2)])
        k_r = Ring([Tile("k%d" % i, kT[:, i]) for i in range(2)])
        v_r = Ring([Tile("v%d" % i, vT[:, i]) for i in range(2)])
        p_r = Ring([Tile("p%d" % i, pT[:, i]) for i in range(4)])
        rd_r = Ring([Tile("rd%d" % i, rdT[:, i]) for i in range(2)])
        oo_r = Ring([Tile("oo%d" % i, ooT[:, i]) for i in range(2)])
        ps = kb.ptiles("ob")
        st_r = Ring([ps[0], ps[1], ps[2], ps[3]])
        o_r = Ring([ps[4], ps[5]])
        d_r = Ring([ps[6], ps[7]])
        sm = 128.0 ** -0.5
        LA = 2
        ld = {}
        cur = {}

        def load_hg(hh, g):
            qt, kt, vt = q_r.next(), k_r.next(), v_r.next()
            sc.op("sp", lambda e: e.dma_start(out=qt.ap, in_=qk_s[g, 0, hh]),
                  reads=[kb.qk_t[g][0][hh]], writes=[qt], dma=qt.name)
            sc.op("sp", lambda e: e.dma_start(out=kt.ap, in_=qk_s[g, 1, hh]),
                  reads=[kb.qk_t[g][1][hh]], writes=[kt], dma=qt.name)
            sc.op("sp", lambda e: e.dma_start(
                out=vt.ap, in_=v_s[g].rearrange("(b p) c -> p b c", p=128)[:, :, hh * 128:(hh + 1) * 128]),
                reads=[kb.v_t[g]], writes=[vt], dma=qt.name)
            ld[(hh, g)] = (qt, kt, vt)

        def front(hh, g, pb4, half):
            if (hh, g) not in ld:
                load_hg(hh, g)
            qt, kt, vt = ld[(hh, g)]
            d = DILS[g]
            stp = st_r.next()
            pbs = (pb4 + 2 * half, pb4 + 2 * half + 1)
            for bi, pb in enumerate(pbs):
                hasprev = (pb // d) > 0
                pv = (pb - d) if hasprev else pb
                sc.op("pe", lambda e, bi=bi, pb=pb, pv=pv: e.matmul(
                    stp.ap[:, bi * 256:bi * 256 + 128], kt.ap[:, pv * 128:(pv + 1) * 128],
                    qt.ap[:, pb * 128:(pb + 1) * 128], start=True, stop=True), reads=[kt, qt], writes=[stp])
                sc.op("pe", lambda e, bi=bi, pb=pb: e.matmul(
                    stp.ap[:, bi * 256 + 128:bi * 256 + 256], kt.ap[:, pb * 128:(pb + 1) * 128],
                    qt.ap[:, pb * 128:(pb + 1) * 128], start=True, stop=True), reads=[kt, qt], writes=[stp])
            pt = p_r.next()
            sc.op("act", lambda e: e.activation(pt.ap, stp.ap, AF.Exp, scale=sm), reads=[stp], writes=[pt])
            sc.op("pool", lambda e: e.tensor_tensor(pt.ap, pt.ap, kb.omask_t[:], ALU.mult),
                  reads=[pt, kb.omask], writes=[pt])
            return pt

        def back(hh, g, pb4, half, pt):
            qt, kt, vt = ld[(hh, g)]
            d = DILS[g]
            if half == 0:
                cur["o"], cur["d"] = o_r.next(), d_r.next()
            op_, dp_ = cur["o"], cur["d"]
            pbs = (pb4 + 2 * half, pb4 + 2 * half + 1)
            for bi, pb in enumerate(pbs):
                hasprev = (pb // d) > 0
                slot = 2 * half + bi
                for (dst, isden) in ((op_, False), (dp_, True)):
                    if hasprev:
                        sc.op("pe", lambda e, dst=dst, isden=isden, slot=slot, pb=pb, bi=bi: e.matmul(
                            dst.ap[:, slot * 128:(slot + 1) * 128],
                            kb.onesb_t[:] if isden else vt.ap[:, pb - d, :],
                            pt.ap[:, bi * 256:bi * 256 + 128], start=True, stop=False),
                            reads=[pt, vt, kb.onesb], writes=[dst])
                    sc.op("pe", lambda e, dst=dst, isden=isden, slot=slot, pb=pb, bi=bi, hasprev=hasprev: e.matmul(
                        dst.ap[:, slot * 128:(slot + 1) * 128],
                        kb.onesb_t[:] if isden else vt.ap[:, pb, :],
                        pt.ap[:, bi * 256 + 128:bi * 256 + 256], start=(not hasprev), stop=True),
                        reads=[pt, vt, kb.onesb], writes=[dst])
            accO, accD, ao_t, ad_t = accO2[:, hh % 2], accD2[:, hh % 2], ao_t2[hh % 2], ad_t2[hh % 2]
            if half == 1:
                for (src, acc, acc_t) in ((op_, accO, ao_t), (dp_, accD, ad_t)):
                    if g == 0:
                        sc.op("act", lambda e, src=src, acc=acc: e.activation(
                            acc[:, pb4 * 128:(pb4 + 4) * 128], src.ap, AF.Copy), reads=[src], writes=[acc_t])
                    else:
                        if g == 1:
                            av = lambda acc=acc: acc[:, pb4 * 128:(pb4 + 4) * 128].rearrange("p (i r) -> p r i", r=4)
                        else:
                            n, r0 = pb4 // 16, pb4 % 16
                            av = lambda acc=acc, n=n, r0=r0: acc[:, n * 2048:(n + 1) * 2048].rearrange(
                                "p (i r) -> p r i", r=16)[:, r0:r0 + 4, :]
                        sc.op("dve", lambda e, src=src, av=av: e.tensor_tensor(
                            av(), src.ap.rearrange("p (r i) -> p r i", r=4), av(), ALU.add), reads=[src, acc_t], writes=[acc_t])
                if g == 2 and pb4 == NB - 4:
                    for tt in range(NT):
                        rd = rd_r.next()
                        oo = oo_r.next()
                        sc.op("act", lambda e, rd=rd, tt=tt: e.activation(rd.ap, accD[:, tt * TT:(tt + 1) * TT], AF.Ln), reads=[ad_t], writes=[rd])
                        sc.op("act", lambda e, rd=rd: e.activation(rd.ap, rd.ap, AF.Exp, scale=-1.0), reads=[rd], writes=[rd])
                        sc.op("dve", lambda e, rd=rd, oo=oo, tt=tt: e.tensor_tensor(oo.ap, accO[:, tt * TT:(tt + 1) * TT], rd.ap, ALU.mult),
                              reads=[ao_t, rd], writes=[oo])
                        sc.op("act", lambda e, oo=oo, tt=tt: e.dma_start(out=od[hh * 128:(hh + 1) * 128, tt * TT:(tt + 1) * TT], in_=oo.ap),
                              reads=[oo], writes=[kb.od_t[tt]], dma="st_" + oo.name)

        steps = [(hh, g, pb4, half) for hh in range(4) for g in range(3) for pb4 in range(0, NB, 4) for half in range(2)]
        pend = []
        for st in steps:
            pend.append(st + (front(*st),))
            if len(pend) > LA:
                back(*pend.pop(0))
        while pend:
            back(*pend.pop(0))
        kb.barrier()


EVEN_IN = 3088
C_AQ, C_AK, C_AV, C_AG, C_AR, C_BQ, C_BK, C_BV = 0, 256, 512, 1024, 1536, 1552, 2064, 2576


def phase_even_a(kb, L, Le, x_src, xs_t, w_in, wa2_d, ba_d, sd, vec, hook=None):
    nc, sc, S, NT = kb.nc, kb.sc, kb.S, kb.NT
    V = kb.VOFF
    with ExitStack() as pes:
        wT = kb.sb(pes, "ea_w", [128, 8, EVEN_IN], BF16)
        w_bt = load_cast_weight(kb, wT, "ea_w", w_in[Le].rearrange("(kc p) n -> p kc n", p=128), EVEN_IN)
        wa2T = kb.sb(pes, "ea_wa2", [16, 256], F32)
        baT = kb.sb(pes, "ea_ba", [1, 256], F32)
        wa_t = Tile("ea_wa", wa2T[:])
        sc.op("sp", lambda e: e.dma_start(out=wa2T[:], in_=wa2_d[Le]), writes=[wa_t], dma="ea_wa")
        sc.op("sp", lambda e: e.dma_start(out=baT[:], in_=ba_d[Le]), writes=[wa_t], dma="ea_wa")
        xT = kb.sb(pes, "ea_x", [128, 2, 8, TT], F32)
        h0T = kb.sb(pes, "ea_h0", [128, 2, 8, TT], BF16)
        sqT = kb.sb(pes, "ea_sq", [128, 2, TT], F32)
        lnT = kb.sb(pes, "ea_ln", [128, TT], F32)
        rsT = kb.sb(pes, "ea_rs", [128, TT], F32)
        sq2T = kb.sb(pes, "ea_sq2", [128, 2, TT], F32)
        ln2T = kb.sb(pes, "ea_ln2", [128, 2, TT], F32)
        rs2T = kb.sb(pes, "ea_rs2", [128, 2, TT], F32)
        arT = kb.sb(pes, "ea_ar", [16, TT], F32)
        e1T = kb.sb(pes, "ea_e1", [128, 2, 256], F32)
        spT = kb.sb(pes, "ea_sp", [128, 2, 256], F32)
        erT = kb.sb(pes, "ea_er", [128, 2, 256], F32)
        ebT = kb.sb(pes, "ea_eb", [128, 2, TT], F32)
        enbT = kb.sb(pes, "ea_enb", [128, 2, TT], F32)
        dcT = kb.sb(pes, "ea_dc", [128, 2, 8], F32)
        foT = kb.sb(pes, "ea_fo", [128, 4, TT], BF16)
        toT = kb.sb(pes, "ea_to", [128, 3, TT], BF16)
        keT = kb.sb(pes, "ea_ke", [128, 2, 256], BF16)
        x_r = Ring([Tile("x%d" % i, xT[:, i]) for i in range(2)])
        h0_r = Ring([Tile("h0%d" % i, h0T[:, i]) for i in range(2)])
        sq_r = Ring([Tile("sq%d" % i, sqT[:, i]) for i in range(2)])
        ln_t = Tile("ln", lnT[:])
        rs_t = Tile("rs", rsT[:])
        sq2_r = Ring([Tile("sq2%d" % i, sq2T[:, i]) for i in range(2)])
        ln2_r = Ring([Tile("ln2%d" % i, ln2T[:, i]) for i in range(2)])
        rs2_r = Ring([Tile("rs2%d" % i, rs2T[:, i]) for i in range(2)])
        ar_t = Tile("ar", arT[:])
        e1_r = Ring([Tile("e1%d" % i, e1T[:, i]) for i in range(2)])
        sp_r = Ring([Tile("sp%d" % i, spT[:, i]) for i in range(2)])
        er_r = Ring([Tile("er%d" % i, erT[:, i]) for i in range(2)])
        eb_t = [Tile("eb%d" % i, ebT[:, i]) for i in range(2)]
        enb_t = [Tile("enb%d" % i, enbT[:, i]) for i in range(2)]
        dc_r = Ring([Tile("dc%d" % i, dcT[:, i]) for i in range(2)])
        fo_r = Ring([Tile("fo%d" % i, foT[:, i]) for i in range(4)])
        to_r = Ring([Tile("to%d" % i, toT[:, i]) for i in range(3)])
        ke_r = Ring([Tile("ke%d" % i, keT[:, i]) for i in range(2)])
        ps = kb.ptiles("ea")
        z_r = Ring([ps[1], ps[2], ps[0]])
        bt_ps = [ps[3], ps[4]]
        s_r = Ring([ps[5], ps[6]])
        tk_r = Ring([ps[7]])
        xs_v = x_src.rearrange("(c p) t -> p c t", p=128)

        def load_tile(tt):
            xt = x_r.next()
            sc.op("sp", lambda e, xt=xt, tt=tt: e.dma_start(out=xt.ap, in_=xs_v[:, :, tt * TT:(tt + 1) * TT]),
                  reads=[xs_t[tt]], writes=[xt], dma=xt.name)
            return xt

        def fm_proj(h0, col, M=128):
            zp = z_r.next()
            for kc in range(8):
                sc.op("pe", lambda e, zp=zp, kc=kc, h0=h0: e.matmul(
                    zp.ap[0:M, :], wT[:, kc, col:col + M], h0.ap[:, kc], start=(kc == 0), stop=(kc == 7)),
                    reads=wblk(w_bt, col, col + M) + [h0], writes=[zp])
            return zp

        def store_fm(fo, dst_ap, dst_tile):
            sc.op("sp", lambda e: e.dma_start(out=dst_ap, in_=fo.ap), reads=[fo], writes=[dst_tile], dma="st_" + fo.name)

        nxt = load_tile(0)
        for tt in range(NT):
            xt = nxt
            if tt + 1 < NT:
                nxt = load_tile(tt + 1)
            t0, t1 = tt * TT, (tt + 1) * TT
            if hook is not None:
                hook(tt)
            ss_ps = s_r.next()
            norm_tile(kb, xt, ss_ps, sq_r, lnT, ln_t, rsT, rs_t)
            h0 = h0_r.next()
            for c in range(8):
                gc = V["nm"](L) + c
                sc.op("dve", lambda e, c=c, gc=gc, h0=h0, xt=xt: e.scalar_tensor_tensor(
                    h0.ap[:, c], xt.ap[:, c], vec.ap[:, gc:gc + 1], rsT[:], ALU.mult, ALU.mult),
                    reads=[xt, rs_t, vec], writes=[h0])
            zp = fm_proj(h0, C_AR, M=16)
            sc.op("act", lambda e, zp=zp: e.activation(arT[:], zp.ap[0:16, :], AF.Copy), reads=[zp], writes=[ar_t])
            for sub in range(4):
                c0, c1 = sub * 128, (sub + 1) * 128
                la = s_r.next()
                sc.op("pe", lambda e, la=la, c0=c0, c1=c1: e.matmul(la.ap[:, 0:256], arT[0:16, c0:c1], wa2T[:], start=True, stop=False),
                      reads=[ar_t, wa_t], writes=[la])
                sc.op("pe", lambda e, la=la: e.matmul(la.ap[:, 0:256], kb.ones32_t[0:1, :], baT[:], start=False, stop=True),
                      reads=[kb.ones32, wa_t], writes=[la])
                e1 = e1_r.next()
                sc.op("act", lambda e, e1=e1, la=la: e.activation(e1.ap, la.ap[:, 0:256], AF.Exp, scale=-1.0), reads=[la], writes=[e1])
                sp = sp_r.next()
                sc.op("act", lambda e, e1=e1, sp=sp: e.activation(sp.ap, e1.ap, AF.Ln, bias=kb.one_t[:], scale=1.0),
                      reads=[e1, kb.one], writes=[sp])
                for fc in range(2):
                    sc.op("pe", lambda e, fc=fc, sp=sp, c0=c0, c1=c1: e.matmul(
                        bt_ps[fc].ap[:, c0:c1], sp.ap[:, fc * 128:(fc + 1) * 128], kb.umask_t[:], start=True, stop=True),
                        reads=[sp, kb.gmasks], writes=[bt_ps[fc]])
                br = s_r.next()
                sc.op("pe", lambda e, br=br, sp=sp: e.matmul(br.ap[:, 0:256], kb.lmask_t[:], sp.ap, start=True, stop=True),
                      reads=[sp, kb.gmasks], writes=[br])
                er = er_r.next()
                sc.op("act", lambda e, er=er, br=br: e.activation(er.ap, br.ap[:, 0:256], AF.Exp), reads=[br], writes=[er])
                for (col, ncol, kind) in ((C_AK, 256, "ke"), (C_AV, 512, "av"), (C_BV, 512, "bv")):
                    tp = tk_r.next()
                    for kc in range(8):
                        sc.op("pe", lambda e, tp=tp, kc=kc, c0=c0, c1=c1, col=col, ncol=ncol, h0=h0: e.matmul(
                            tp.ap[:, 0:ncol], h0.ap[:, kc, c0:c1], wT[:, kc, col:col + ncol], start=(kc == 0), stop=(kc == 7)),
                            reads=wblk(w_bt, col, col + ncol) + [h0], writes=[tp])
                    if kind == "ke":
                        ke = ke_r.next()
                        sc.op("dve", lambda e, ke=ke, tp=tp, er=er: e.tensor_tensor(ke.ap, tp.ap[:, 0:256], er.ap, ALU.mult),
                              reads=[tp, er], writes=[ke])
                        sc.op("sp", lambda e, ke=ke, c0=c0, c1=c1, t0=t0: e.dma_start(out=sd["gke"][t0 + c0:t0 + c1, :], in_=ke.ap),
                              reads=[ke], writes=[kb.sd_t["gke"]], dma="st_" + ke.name)
                    else:
                        to = to_r.next()
                        sc.op("act", lambda e, to=to, tp=tp: e.activation(to.ap, tp.ap, AF.Copy), reads=[tp], writes=[to])
                        nm = "gv" if kind == "av" else "dv"
                        sc.op("sp", lambda e, to=to, c0=c0, c1=c1, t0=t0, nm=nm: e.dma_start(out=sd[nm][t0 + c0:t0 + c1, :], in_=to.ap),
                              reads=[to], writes=[kb.sd_t[nm]], dma="st_" + to.name)
            for fc in range(2):
                sc.op("act", lambda e, fc=fc: e.activation(ebT[:, fc], bt_ps[fc].ap, AF.Exp), reads=[bt_ps[fc]], writes=[eb_t[fc]])
                sc.op("act", lambda e, fc=fc: e.activation(enbT[:, fc], bt_ps[fc].ap, AF.Exp, scale=-1.0), reads=[bt_ps[fc]], writes=[enb_t[fc]])
                dc = dc_r.next()
                sc.op("dve", lambda e, fc=fc, dc=dc: e.tensor_copy(dc.ap, ebT[:, fc].rearrange("p (n c) -> p n c", c=64)[:, :, 63]),
                      reads=[eb_t[fc]], writes=[dc])
                sc.op("sp", lambda e, fc=fc, dc=dc, tt=tt: e.dma_start(out=sd["gdec"][fc * 128:(fc + 1) * 128, tt * 8:(tt + 1) * 8], in_=dc.ap),
                      reads=[dc], writes=[kb.sd_t["gdec"]], dma="st_" + dc.name)
            for fc in range(2):
                zp = fm_proj(h0, C_AQ + fc * 128)
                fo = fo_r.next()
                sc.op("dve", lambda e, zp=zp, fo=fo, fc=fc: e.scalar_tensor_tensor(fo.ap, zp.ap, 0.125, ebT[:, fc], ALU.mult, ALU.mult),
                      reads=[zp, eb_t[fc]], writes=[fo])
                store_fm(fo, sd["gq"][fc * 128:(fc + 1) * 128, t0:t1], kb.sd_t["gq"])
                zp = fm_proj(h0, C_AK + fc * 128)
                fo = fo_r.next()
                sc.op("dve", lambda e, zp=zp, fo=fo, fc=fc: e.tensor_tensor(fo.ap, zp.ap, enbT[:, fc], ALU.mult),
                      reads=[zp, enb_t[fc]], writes=[fo])
                store_fm(fo, sd["gk"][fc * 128:(fc + 1) * 128, t0:t1], kb.sd_t["gk"])
            for c in range(4):
                zp = fm_proj(h0, C_AG + c * 128)
                fo = fo_r.next()
                sc.op("act", lambda e, zp=zp, fo=fo: e.activation(fo.ap, zp.ap, AF.Silu), reads=[zp], writes=[fo])
                store_fm(fo, sd["gg"][c * 128:(c + 1) * 128, t0:t1], kb.sd_t["gg"])
            def finish_f(zp, sq, gcol, nm, c):
                sp2 = s_r.next()
                sc.op("pe", lambda e: e.matmul(sp2.ap, r32(kb.bd64r_t[:]), r32(sq.ap), start=True, stop=True),
                      reads=[sq, kb.gmasks], writes=[sp2])
                ln = ln2_r.next()
                sc.op("act", lambda e: e.activation(ln.ap, sp2.ap, AF.Ln, bias=kb.eps_t[:], scale=1.0 / 64),
                      reads=[sp2, kb.eps], writes=[ln])
                rs = rs2_r.next()
                sc.op("act", lambda e: e.activation(rs.ap, ln.ap, AF.Exp, scale=-0.5), reads=[ln], writes=[rs])
                fo = fo_r.next()
                sc.op("dve", lambda e: e.scalar_tensor_tensor(
                    fo.ap, zp.ap, vec.ap[:, gcol:gcol + 1], rs.ap, ALU.mult, ALU.mult), reads=[zp, rs, vec], writes=[fo])
                store_fm(fo, sd[nm][c * 128:(c + 1) * 128, t0:t1], kb.sd_t[nm])

            pend = []
            for (cbase, gcol, nm) in ((C_BQ, V["eg"](Le) + 1, "dq"), (C_BK, V["eg"](Le) + 2, "dk")):
                for c in range(4):
                    zp = fm_proj(h0, cbase + c * 128)
                    sq = sq2_r.next()
                    sc.op("act", lambda e, sq=sq, zp=zp: e.activation(r32(sq.ap), zp.ap, AF.Square), reads=[zp], writes=[sq])
                    pend.append((zp, sq, gcol, nm, c))
                    if len(pend) > 1:
                        finish_f(*pend.pop(0))
            finish_f(*pend.pop(0))
        kb.barrier()


def head_norm_store(kb, src_ap, src_tiles, sq_r, ss_ps, ln_r, rs_r, gain_ap, gain_tiles, mul_tile, out_r, dst_ap, dst_tile, tmp_r=None):
    sc = kb.sc
    sq = sq_r.next()
    sc.op("act", lambda e: e.activation(r32(sq.ap), src_ap, AF.Square), reads=src_tiles, writes=[sq])
    sc.op("pe", lambda e: e.matmul(ss_ps.ap, r32(kb.ones32_t[:]), r32(sq.ap), start=True, stop=True), reads=[sq, kb.ones32], writes=[ss_ps])
    ln = ln_r.next()
    sc.op("act", lambda e: e.activation(ln.ap, ss_ps.ap, AF.Ln, bias=kb.eps_t[:], scale=1.0 / 128), reads=[ss_ps, kb.eps], writes=[ln])
    rs = rs_r.next()
    sc.op("act", lambda e: e.activation(rs.ap, ln.ap, AF.Exp, scale=-0.5), reads=[ln], writes=[rs])
    oo = out_r.next()
    if mul_tile is None:
        sc.op("dve", lambda e: e.scalar_tensor_tensor(oo.ap, src_ap, gain_ap, rs.ap, ALU.mult, ALU.mult),
              reads=src_tiles + [rs] + gain_tiles, writes=[oo])
    else:
        tmp = tmp_r.next()
        sc.op("dve", lambda e: e.scalar_tensor_tensor(tmp.ap, src_ap, gain_ap, rs.ap, ALU.mult, ALU.mult),
              reads=src_tiles + [rs] + gain_tiles, writes=[tmp])
        sc.op("pool", lambda e: e.tensor_tensor(oo.ap, tmp.ap, mul_tile.ap, ALU.mult), reads=[tmp, mul_tile], writes=[oo])
    sc.op("act", lambda e: e.dma_start(out=dst_ap, in_=oo.ap), reads=[oo], writes=[dst_tile], dma="st_" + oo.name)


def phase_even_gla(kb, Le, sd, od, vec):
    nc, sc, S, NT = kb.nc, kb.sc, kb.S, kb.NT
    NB = S // 128
    V = kb.VOFF
    with ExitStack() as pes:
        gqT = kb.sb(pes, "eg_q", [128, 2, S], BF16)
        gkT = kb.sb(pes, "eg_k", [128, 2, S], BF16)
        gkeT = kb.sb(pes, "eg_ke", [128, NB, 256], BF16)
        gvT = kb.sb(pes, "eg_v", [128, NB, 512], BF16)
        decT = kb.sb(pes, "eg_dec", [128, 2, S // 64], F32)
        ggT = kb.sb(pes, "eg_gg", [128, 3, TT], BF16)
        s32T = kb.sb(pes, "eg_s32", [128, 2, 128], F32)
        sbfT = kb.sb(pes, "eg_sbf", [128, 2, 4, 128], BF16)
        attT = kb.sb(pes, "eg_att", [128, 4, 128], BF16)
        sqT = kb.sb(pes, "eg_sq", [128, 2, TT], F32)
        lnT = kb.sb(pes, "eg_ln", [128, 2, TT], F32)
        rsT = kb.sb(pes, "eg_rs", [128, 2, TT], F32)
        tmT = kb.sb(pes, "eg_tm", [128, 2, TT], F32)
        ooT = kb.sb(pes, "eg_oo", [128, 3, TT], BF16)
        in_t = Tile("eg_in", None)
        for fc in range(2):
            sc.op("sp", lambda e, fc=fc: e.dma_start(out=gqT[:, fc], in_=sd["gq"][fc * 128:(fc + 1) * 128, :]),
                  reads=[kb.sd_t["gq"]], writes=[in_t], dma="eg_in")
            sc.op("sp", lambda e, fc=fc: e.dma_start(out=gkT[:, fc], in_=sd["gk"][fc * 128:(fc + 1) * 128, :]),
                  reads=[kb.sd_t["gk"]], writes=[in_t], dma="eg_in")
            sc.op("sp", lambda e, fc=fc: e.dma_start(out=decT[:, fc], in_=sd["gdec"][fc * 128:(fc + 1) * 128, :]),
                  reads=[kb.sd_t["gdec"]], writes=[in_t], dma="eg_in")
        sc.op("sp", lambda e: e.dma_start(out=gkeT[:], in_=sd["gke"].rearrange("(b p) c -> p b c", p=128)),
              reads=[kb.sd_t["gke"]], writes=[in_t], dma="eg_in")
        for q4 in range(4):
            sc.op("sp", lambda e, q4=q4: e.dma_start(out=gvT[:, q4 * (NB // 4):(q4 + 1) * (NB // 4)],
                  in_=sd["gv"].rearrange("(b p) c -> p b c", p=128)[:, q4 * (NB // 4):(q4 + 1) * (NB // 4)]),
                  reads=[kb.sd_t["gv"]], writes=[in_t], dma="eg_in")
        gg_r = Ring([Tile("gg%d" % i, ggT[:, i]) for i in range(3)])
        s32_t = [Tile("s32%d" % i, s32T[:, i]) for i in range(2)]
        sbf_r = [Ring([Tile("sbf%d_%d" % (p, i), sbfT[:, p, i]) for i in range(4)]) for p in range(2)]
        att_r = Ring([Tile("att%d" % i, attT[:, i]) for i in range(4)])
        sq_r = Ring([Tile("sq%d" % i, sqT[:, i]) for i in range(2)])
        ln_r = Ring([Tile("ln%d" % i, lnT[:, i]) for i in range(2)])
        rs_r = Ring([Tile("rs%d" % i, rsT[:, i]) for i in range(2)])
        tm_r = Ring([Tile("tm%d" % i, tmT[:, i]) for i in range(2)])
        oo_r = Ring([Tile("oo%d" % i, ooT[:, i]) for i in range(3)])
        ps = kb.ptiles("eg")
        o_ps = [ps[0], ps[1], ps[2], ps[3]]
        at_b = [ps[4], ps[5]]
        kv_b = [ps[6], ps[7]]
        ss_ps = ps[4]
        cur = []
        for p in range(2):
            sc.op("pool", lambda e, p=p: e.memset(s32T[:, p], 0.0), writes=[s32_t[p]])
            sb0 = sbf_r[p].next()
            sc.op("pool", lambda e, sb0=sb0: e.memset(sb0.ap, 0.0), writes=[sb0])
            cur.append(sb0)
        for tt in range(NT):
            for sub in range(4):
                sbk = tt * 4 + sub
                k0, k1 = sbk * 128, (sbk + 1) * 128
                atts = []
                for h in range(4):
                    fc, po = h // 2, (h % 2) * 64
                    ap_ = at_b[h % 2]
                    sc.op("pe", lambda e, ap_=ap_, fc=fc, po=po, k0=k0, k1=k1: e.matmul(
                        ap_.ap[:, fc * 128:(fc + 1) * 128], gkT[po:po + 64, fc, k0:k1], gqT[po:po + 64, fc, k0:k1], start=True, stop=True),
                        reads=[in_t], writes=[ap_])
                    at = att_r.next()
                    sc.op("dve", lambda e, at=at, ap_=ap_, fc=fc: e.tensor_tensor(at.ap, ap_.ap[:, fc * 128:(fc + 1) * 128], kb.gmask_t[:], ALU.mult),
                          reads=[ap_, kb.gmasks], writes=[at])
                    atts.append(at)
                s_c1 = []
                nxt_state = []
                for p in range(2):
                    sts = [cur[p]]
                    for c in range(2):
                        for hp in range(2):
                            h = p * 2 + hp
                            po = hp * 64
                            sc.op("pe", lambda e, c=c, h=h, po=po, sbk=sbk, p=p: e.matmul(
                                kv_b[c].ap[po:po + 64, p * 128:(p + 1) * 128], gkeT[c * 64:(c + 1) * 64, sbk, h * 64:(h + 1) * 64],
                                gvT[c * 64:(c + 1) * 64, sbk, h * 128:(h + 1) * 128], start=True, stop=True),
                                reads=[in_t], writes=[kv_b[c]])
                        ch = sbk * 2 + c
                        sc.op("dve", lambda e, p=p, c=c, ch=ch: e.scalar_tensor_tensor(
                            s32T[:, p], s32T[:, p], decT[:, p, ch:ch + 1], kv_b[c].ap[:, p * 128:(p + 1) * 128], ALU.mult, ALU.add),
                            reads=[s32_t[p], kv_b[c], in_t], writes=[s32_t[p]])
                        sbn = sbf_r[p].next()
                        sc.op("act", lambda e, p=p, sbn=sbn: e.activation(sbn.ap, s32T[:, p], AF.Copy), reads=[s32_t[p]], writes=[sbn])
                        sts.append(sbn)
                    s_c1.append(sts)
                    nxt_state.append(sts[2])
                for h in range(4):
                    fc, po, p = h // 2, (h % 2) * 64, h // 2
                    st0, st1 = s_c1[p][0], s_c1[p][1]
                    at = atts[h]
                    oc0 = sub * 128
                    sc.op("pe", lambda e, h=h, at=at, sbk=sbk, oc0=oc0: e.matmul(
                        o_ps[h].ap[:, oc0:oc0 + 128], gvT[:, sbk, h * 128:(h + 1) * 128], at.ap, start=True, stop=False),
                        reads=[in_t, at], writes=[o_ps[h]])
                    sc.op("pe", lambda e, h=h, st0=st0, fc=fc, po=po, k0=k0, oc0=oc0: e.matmul(
                        o_ps[h].ap[:, oc0:oc0 + 64], st0.ap[po:po + 64, :], gqT[po:po + 64, fc, k0:k0 + 64], start=False, stop=False),
                        reads=[in_t, st0], writes=[o_ps[h]])
                    sc.op("pe", lambda e, h=h, st1=st1, fc=fc, po=po, k0=k0, oc0=oc0: e.matmul(
                        o_ps[h].ap[:, oc0 + 64:oc0 + 128], st1.ap[po:po + 64, :], gqT[po:po + 64, fc, k0 + 64:k0 + 128], start=False, stop=True),
                        reads=[in_t, st1], writes=[o_ps[h]])
                cur = nxt_state
            for h in range(4):
                gg = gg_r.next()
                sc.op("sp", lambda e, gg=gg, h=h, tt=tt: e.dma_start(out=gg.ap, in_=sd["gg"][h * 128:(h + 1) * 128, tt * TT:(tt + 1) * TT]),
                      reads=[kb.sd_t["gg"]], writes=[gg], dma=gg.name)
                gcol = V["eg"](Le)
                head_norm_store(kb, o_ps[h].ap, [o_ps[h]], sq_r, at_b[0], ln_r, rs_r, vec.ap[:, gcol:gcol + 1], [vec], gg, oo_r,
                                od[h * 128:(h + 1) * 128, tt * TT:(tt + 1) * TT], kb.od_t[tt], tmp_r=tm_r)
        kb.barrier()


def phase_even_diff(kb, L, Le, sd, od, vec, lvec_d):
    nc, sc, S, NT = kb.nc, kb.sc, kb.S, kb.NT
    NB = S // 128
    V = kb.VOFF
    lam_init = 0.8 - 0.6 * float(np.exp(-0.3 * L))
    with ExitStack() as pes:
        dqT = kb.sb(pes, "ed_q", [128, 2, 2, S], BF16)
        dkT = kb.sb(pes, "ed_k", [128, 2, S], BF16)
        dvT = kb.sb(pes, "ed_v", [128, 2, NB, 128], BF16)
        pT = kb.sb(pes, "ed_p", [128, 6, TT], BF16)
        lvT = kb.sb(pes, "ed_lv", [128, 4, 64], F32)
        ltT = kb.sb(pes, "ed_lt", [128, 2, 64], F32)
        lsT = kb.sb(pes, "ed_ls", [128, 8], F32)
        rT = kb.sb(pes, "ed_r", [128, 2, TT], F32)
        t1T = kb.sb(pes, "ed_t1", [128, 2, TT], F32)
        oT = kb.sb(pes, "ed_o", [128, 2, TT], F32)
        sqT = kb.sb(pes, "ed_sq", [128, 2, TT], F32)
        lnT = kb.sb(pes, "ed_ln", [128, 2, TT], F32)
        rsT = kb.sb(pes, "ed_rs", [128, 2, TT], F32)
        ooT = kb.sb(pes, "ed_oo", [128, 3, TT], BF16)
        q_r = Ring([Tile("q%d" % i, dqT[:, i]) for i in range(2)])
        for i_ in range(2):
            sc.op("pool", lambda e, i_=i_: e.memset(dqT[64:128, i_, 0, :], 0.0), writes=[q_r.tiles[i_]])
            sc.op("pool", lambda e, i_=i_: e.memset(dqT[0:64, i_, 1, :], 0.0), writes=[q_r.tiles[i_]])
        k_r = Ring([Tile("k%d" % i, dkT[:, i]) for i in range(2)])
        v_r = Ring([Tile("v%d" % i, dvT[:, i]) for i in range(2)])
        p_r = Ring([Tile("p%d" % i, pT[:, i]) for i in range(6)])
        r_r = Ring([Tile("r%d" % i, rT[:, i]) for i in range(2)])
        t1_r = Ring([Tile("t1%d" % i, t1T[:, i]) for i in range(2)])
        o_r = Ring([Tile("o%d" % i, oT[:, i]) for i in range(2)])
        sq_r = Ring([Tile("sq%d" % i, sqT[:, i]) for i in range(2)])
        ln_r = Ring([Tile("ln%d" % i, lnT[:, i]) for i in range(2)])
        rs_r = Ring([Tile("rs%d" % i, rsT[:, i]) for i in range(2)])
        oo_r = Ring([Tile("oo%d" % i, ooT[:, i]) for i in range(3)])
        ps = kb.ptiles("ed")
        st_r = Ring([ps[0], ps[1], ps[2], ps[3]])
        acc = [[ps[4], ps[5]], [ps[6], ps[7]]]
        lv_t = Tile("lv", lvT[:])
        ls_t = Tile("ls", lsT[:])
        sc.op("sp", lambda e: e.dma_start(out=lvT[:], in_=lvec_d[Le]), writes=[lv_t], dma="ed_lv")
        for i in range(2):
            sc.op("dve", lambda e, i=i: e.tensor_tensor(ltT[:, i], lvT[:, 2 * i], lvT[:, 2 * i + 1], ALU.mult), reads=[lv_t], writes=[ls_t])
            sc.op("dve", lambda e, i=i: e.reduce_sum(lsT[:, i:i + 1], ltT[:, i], AX.X), reads=[ls_t], writes=[ls_t])
            sc.op("act", lambda e, i=i: e.activation(lsT[:, 2 + i:3 + i], lsT[:, i:i + 1], AF.Exp), reads=[ls_t], writes=[ls_t])
        sc.op("dve", lambda e: e.tensor_tensor(lsT[:, 4:5], lsT[:, 3:4], lsT[:, 2:3], ALU.subtract), reads=[ls_t], writes=[ls_t])
        sc.op("dve", lambda e: e.tensor_scalar(lsT[:, 4:5], lsT[:, 4:5], -lam_init, None, ALU.add), reads=[ls_t], writes=[ls_t])
        gcol = V["eg"](Le) + 3
        sc.op("dve", lambda e: e.tensor_scalar(lsT[:, 5:6], vec.ap[:, gcol:gcol + 1], 1.0 - lam_init, None, ALU.mult),
              reads=[ls_t, vec], writes=[ls_t])
        LA = 3
        hd = {}
        pairq = {0: [], 1: []}
        denq = []
        smT = kb.sb(pes, "ed_sm", [128, 4, TT], BF16)
        sm_r = Ring([Tile("sm%d" % i, smT[:, i]) for i in range(4)])

        def load_head(h):
            qt, kt, vt = q_r.next(), k_r.next(), v_r.next()
            sc.op("sp", lambda e: e.dma_start(out=qt.ap[0:64, 0, :], in_=sd["dq"][h * 128:h * 128 + 64, :]),
                  reads=[kb.sd_t["dq"]], writes=[qt], dma=qt.name)
            sc.op("sp", lambda e: e.dma_start(out=qt.ap[64:128, 1, :], in_=sd["dq"][h * 128 + 64:(h + 1) * 128, :]),
                  reads=[kb.sd_t["dq"]], writes=[qt], dma=qt.name)
            sc.op("sp", lambda e: e.dma_start(out=kt.ap, in_=sd["dk"][h * 128:(h + 1) * 128, :]),
                  reads=[kb.sd_t["dk"]], writes=[kt], dma=qt.name)
            sc.op("sp", lambda e: e.dma_start(
                out=vt.ap, in_=sd["dv"].rearrange("(b p) c -> p b c", p=128)[:, :, h * 128:(h + 1) * 128]),
                reads=[kb.sd_t["dv"]], writes=[vt], dma=qt.name)
            hd[h] = (qt, kt, vt)

        def front(h, qi, kbi, c):
            if h not in hd:
                load_head(h)
            qt, kt, vt = hd[h]
            q0, q1 = qi * TT, (qi + 1) * TT
            k0, k1 = kbi * 128, (kbi + 1) * 128
            stp = st_r.next()
            sc.op("pe", lambda e: e.matmul(
                stp.ap, kt.ap[:, k0:k1], qt.ap[:, c, q0:q1], start=True, stop=True),
                reads=[kt, qt], writes=[stp])
            pt = p_r.next()
            sc.op("act", lambda e: e.activation(pt.ap, stp.ap, AF.Exp, scale=0.125), reads=[stp], writes=[pt])
            dg = kbi - 4 * qi
            if dg >= 0:
                sc.op("pool", lambda e: e.tensor_tensor(pt.ap, pt.ap, kb.dmask_t[:, dg], ALU.mult),
                      reads=[pt, kb.dmask], writes=[pt])
            return pt

        def back(h, qi, kbi, c, pt):
            qt, kt, vt = hd[h]
            nkb = 4 * qi + 4
            sc.op("pe", lambda e: e.matmul(
                acc[c][0].ap, vt.ap[:, kbi, :], pt.ap, start=(kbi == 0), stop=(kbi == nkb - 1)),
                reads=[vt, pt], writes=[acc[c][0]])
            pairq[c].append(pt)
            if len(pairq[c]) == 2:
                p0, p1 = pairq[c]
                pairq[c] = []
                sm = sm_r.next()
                sc.op("dve", lambda e: e.tensor_tensor(sm.ap, p0.ap, p1.ap, ALU.add), reads=[p0, p1], writes=[sm])
                denq.append((c, kbi, nkb, sm))
            last = (kbi == nkb - 1 and c == 1)
            while denq and (last or len(denq) > 2):
                c_, k_, n_, sm_ = denq.pop(0)
                sc.op("pe", lambda e, c_=c_, k_=k_, n_=n_, sm_=sm_: e.matmul(
                    acc[c_][1].ap, kb.onesb_t[:], sm_.ap, start=(k_ == 1), stop=(k_ == n_ - 1)),
                    reads=[kb.onesb, sm_], writes=[acc[c_][1]])
            if last:
                epilogue(h, qi)

        def epilogue(h, qi):
            q0, q1 = qi * TT, (qi + 1) * TT
            ts_ = []
            evs = []
            for c in range(2):
                r = r_r.next()
                sc.op("act", lambda e, r=r, c=c: e.activation(r.ap, acc[c][1].ap, AF.Ln), reads=[acc[c][1]], writes=[r])
                t1 = t1_r.next()
                sc.op("dve", lambda e, t1=t1, c=c: e.tensor_copy(t1.ap, acc[c][0].ap), reads=[acc[c][0]], writes=[t1])
                evs.append((r, t1))
            for c in range(2):
                r, t1 = evs[c]
                sc.op("act", lambda e, r=r: e.activation(r.ap, r.ap, AF.Exp, scale=-1.0), reads=[r], writes=[r])
                sc.op("dve", lambda e, r=r, t1=t1: e.tensor_tensor(t1.ap, t1.ap, r.ap, ALU.mult), reads=[t1, r], writes=[t1])
                ts_.append(t1)
            o = o_r.next()
            sc.op("dve", lambda e, o=o, ta=ts_[0], tb=ts_[1]: e.scalar_tensor_tensor(o.ap, tb.ap, lsT[:, 4:5], ta.ap, ALU.mult, ALU.add),
                  reads=[ts_[0], ts_[1], ls_t], writes=[o])
            ssp = st_r.next()
            head_norm_store(kb, o.ap, [o], sq_r, ssp, ln_r, rs_r, lsT[:, 5:6], [ls_t], None, oo_r,
                            od[512 + h * 128:512 + (h + 1) * 128, q0:q1], kb.od_t[qi])

        steps = [(h, qi, kbi, c) for h in range(4) for qi in range(NT) for kbi in range(4 * qi + 4) for c in range(2)]
        pend = []
        for st in steps:
            pend.append(st + (front(*st),))
            if len(pend) > LA:
                back(*pend.pop(0))
        while pend:
            back(*pend.pop(0))
        kb.barrier()


def host_consts():
    jj = np.arange(128)[:, None]
    ii = np.arange(128)[None, :]
    same = (jj // 64) == (ii // 64)
    c = {}
    mprev = (jj >= ii).astype(np.float32)
    mcur = (jj <= ii).astype(np.float32)
    c["omask"] = np.concatenate([mprev, mcur, mprev, mcur], axis=1)
    qi = np.arange(512)[None, :]
    c["dmask"] = np.concatenate([((g * 128 + jj) <= qi).astype(np.float32) for g in range(4)], axis=1)
    gm = ((jj <= ii) & same).astype(np.float32)
    um = gm * (-1.0 / 16.0)
    lm = ((jj > ii) & same).astype(np.float32) * (-1.0 / 16.0)
    bd = same.astype(np.float32)
    c["gmasks"] = np.concatenate([gm, um, lm, bd], axis=1)
    return c


def setup_consts(kb, es, omask_d, dmask_d, gmasks_d):
    sc = kb.sc
    epsT = kb.sb(es, "eps_sb", [128, 1], F32)
    kb.eps_t = epsT
    kb.eps = Tile("eps", epsT[:])
    sc.op("pool", lambda e: e.memset(epsT[:], EPS), writes=[kb.eps])
    kb.one_t = kb.sb(es, "one_sb", [128, 1], F32)
    kb.one = Tile("one", kb.one_t[:])
    sc.op("pool", lambda e: e.memset(kb.one_t[:], 1.0), writes=[kb.one])
    kb.onesb_t = kb.sb(es, "onesb", [128, 128], BF16)
    kb.onesb = Tile("onesb", kb.onesb_t[:])
    sc.op("pool", lambda e: e.memset(kb.onesb_t[:], 1.0), writes=[kb.onesb])
    kb.omask_t = kb.sb(es, "omask_sb", [128, 512], BF16)
    kb.omask = Tile("omask", kb.omask_t[:])
    sc.op("pool", lambda e: e.dma_start(out=kb.omask_t[:], in_=omask_d), writes=[kb.omask], dma="cst")
    kb.dmask_t = kb.sb(es, "dmask_sb", [128, 4, 512], BF16)
    kb.dmask = Tile("dmask", kb.dmask_t[:])
    sc.op("pool", lambda e: e.dma_start(out=kb.dmask_t[:], in_=dmask_d.rearrange("p (g q) -> p g q", g=4)), writes=[kb.dmask], dma="cst")
    gmT = kb.sb(es, "gmasks_sb", [128, 4, 128], F32)
    kb.gmasks = Tile("gmasks", gmT[:])
    sc.op("sp", lambda e: e.dma_start(out=gmT[:], in_=gmasks_d.rearrange("p (g q) -> p g q", g=4)), writes=[kb.gmasks], dma="cst2")
    kb.gmask_t = gmT[:, 0]
    kb.umask_t = gmT[:, 1]
    kb.lmask_t = gmT[:, 2]
    kb.bd64_t = gmT[:, 3]
    kb.bd64r_t = kb.sb(es, "bd64r", [128, 128], F32)
    sc.op("dve", lambda e: e.tensor_copy(r32(kb.bd64r_t[:]), gmT[:, 3]), reads=[kb.gmasks], writes=[kb.gmasks])


DEPTH = 4
S_FULL = 4096
NVEC = 780
VOFF = {
    "nm": lambda L: L * 192, "nf": lambda L: L * 192 + 8, "cw": lambda L: L * 192 + 16, "cb": lambda L: L * 192 + 148,
    "eg": lambda Le: 768 + Le * 4, "oq": lambda Lo: 776 + Lo * 2,
}


def build_program(S=S_FULL, depth=DEPTH):
    nc = bass.Bass("TRN2", target_bir_lowering=False)
    es = ExitStack()
    kb = KB(nc, es, S)
    NT = kb.NT
    xT = kb.din("xT", [D, S])
    vecs = kb.din("vecs", [128, NVEC])
    lvec = kb.din("lvec", [2, 128, 4, 64])
    wa2 = kb.din("wa2", [2, 16, 256])
    ba = kb.din("ba", [2, 1, 256])
    omask_d = kb.din("omask", [128, 512])
    dmask_d = kb.din("dmask", [128, 2048])
    gmasks_d = kb.din("gmasks", [128, 512])
    ev_w_in = kb.din("ev_w_in", [2, D, EVEN_IN])
    ev_w_out = kb.din("ev_w_out", [2, D, D])
    od_w_in = kb.din("od_w_in", [2, D, ODD_IN])
    od_w_out = kb.din("od_w_out", [2, 512, D])
    w_up = kb.din("ffn_w_up", [4, D, 2 * DFF])
    w_dn = kb.din("ffn_w_down", [4, DFF, D])
    yT = kb.dout("yT", [D, S])
    wup_s = kb.dscr("wup_s", [4, NJ, 128, 2, 8, 128], BF16)
    wdn_s = kb.dscr("wdn_s", [4, 8, 128, NJ, 128], BF16)
    qk_s = kb.dscr("qk_s", [3, 2, 4, 128, S], BF16)
    v_s = kb.dscr("v_s", [3, S, 512], BF16)
    od = kb.dscr("od_s", [1024, S], BF16)
    sd = {}
    kb.sd_t = {}
    for nm, shp, dt in (("gq", [256, S], BF16), ("gk", [256, S], BF16), ("gke", [S, 256], BF16), ("gv", [S, 512], BF16),
                        ("gg", [512, S], BF16), ("gdec", [256, S // 64], F32), ("dq", [512, S], BF16), ("dk", [512, S], BF16),
                        ("dv", [S, 512], BF16)):
        sd[nm] = kb.dscr("s_" + nm, shp, dt)
        kb.sd_t[nm] = Tile("sd_" + nm, None)
    kb.VOFF = VOFF
    kb.wt = {}
    for L in range(4):
        kb.wt[("up", L)] = [Tile("tup%d_%d" % (L, j), None) for j in range(NJ)]
        kb.wt[("dn", L)] = [Tile("tdn%d_%d" % (L, d), None) for d in range(8)]
    vecT = kb.sb(es, "vecs_sb", [128, NVEC], F32)
    vec = Tile("vec", vecT[:])
    kb.sc.op("sp", lambda e: e.dma_start(out=vecT[:], in_=vecs), writes=[vec], dma="vec")
    setup_consts(kb, es, omask_d, dmask_d, gmasks_d)
    xin_t = [Tile("xin%d" % i, None) for i in range(NT)]
    y_t = [Tile("y%d" % i, None) for i in range(NT)]
    kb.od_t = [Tile("od%d" % i, None) for i in range(NT)]
    kb.qk_t = [[[Tile("qk", None) for _ in range(4)] for _ in range(2)] for _ in range(3)]
    kb.v_t = [Tile("v", None) for _ in range(3)]

    def make_hook(th):
        per = -(-len(th) // NT)

        def hook(tt):
            for f in th[tt * per:(tt + 1) * per]:
                f()
        return hook

    for L in range(depth):
        x_src, xs_t = (xT, xin_t) if L == 0 else (yT, y_t)
        hook = make_hook(prep_ffn_weights(kb, L, w_up, w_dn, wup_s, wdn_s))
        if L % 2 == 0:
            Le = L // 2
            phase_even_a(kb, L, Le, x_src, xs_t, ev_w_in, wa2, ba, sd, vec, hook=hook)
            phase_even_gla(kb, Le, sd, od, vec)
            phase_even_diff(kb, L, Le, sd, od, vec, lvec)
            mix = (od, ev_w_out[Le].rearrange("(kc p) n -> p kc n", p=128), 8)
        else:
            Lo = L // 2
            phase_odd_a(kb, L, Lo, x_src, xs_t, od_w_in, qk_s, v_s, vec, hook=hook)
            phase_odd_b(kb, qk_s, v_s, od)
            mix = (od[0:512], od_w_out[Lo].rearrange("(kc p) n -> p kc n", p=128), 4)
        phase_ffn(kb, L, x_src, xs_t, yT, y_t, wup_s, wdn_s, vec, mix=mix)
    kb.sc.emit(final_waits=[(k, c) for k, c in kb.sc.dma_cnt.items() if k.startswith("st_")])
    es.close()
    return nc, kb


def host_vecs(inp):
    v = np.zeros((128, NVEC), np.float32)
    col = lambda a: np.asarray(a, np.float32).reshape(-1, 128).T
    for L in range(4):
        v[:, VOFF["nm"](L):VOFF["nm"](L) + 8] = col(inp["norm_mix"][L])
        v[:, VOFF["nf"](L):VOFF["nf"](L) + 8] = col(inp["norm_ffn"][L])
        cw = np.asarray(inp["ffn_conv_w"][L], np.float32).reshape(3, 2 * DFF)
        for tap in range(3):
            v[:, VOFF["cw"](L) + tap * 44:VOFF["cw"](L) + (tap + 1) * 44] = col(cw[tap])
        v[:, VOFF["cb"](L):VOFF["cb"](L) + 44] = col(inp["ffn_conv_b"][L])
    for e in range(2):
        b = VOFF["eg"](e)
        v[:, b] = inp["ev_gla_gain"][e]
        v[:, b + 1] = np.tile(np.asarray(inp["ev_dq_gain"][e], np.float32), 2)
        v[:, b + 2] = np.tile(np.asarray(inp["ev_dk_gain"][e], np.float32), 2)
        v[:, b + 3] = inp["ev_diff_gain"][e]
    for o in range(2):
        b = VOFF["oq"](o)
        v[:, b] = inp["od_q_gain"][o]
        v[:, b + 1] = inp["od_k_gain"][o]
    return v


_CACHE = {}


def kernel(**inputs):
    inp = {k: np.asarray(v) for k, v in inputs.items()}
    x = inp["x"].astype(np.float32, copy=False)
    B = x.shape[0]
    if "nc" not in _CACHE:
        _CACHE["nc"] = build_program()[0]
    nc = _CACHE["nc"]
    hc = host_consts()
    lv = np.stack([np.stack([inp["ev_lq1"][e], inp["ev_lk1"][e], inp["ev_lq2"][e], inp["ev_lk2"][e]], 0) for e in range(2)], 0)
    lvec = np.ascontiguousarray(np.broadcast_to(lv[:, None].astype(np.float32), (2, 128, 4, 64)))
    shared = {
        "vecs": host_vecs(inp), "lvec": lvec,
        "wa2": np.ascontiguousarray(inp["ev_w_a2"], np.float32),
        "ba": np.ascontiguousarray(inp["ev_b_a"], np.float32).reshape(2, 1, 256),
        "ev_w_in": np.ascontiguousarray(inp["ev_w_in"], np.float32), "ev_w_out": np.ascontiguousarray(inp["ev_w_out"], np.float32),
        "od_w_in": np.ascontiguousarray(inp["od_w_in"], np.float32), "od_w_out": np.ascontiguousarray(inp["od_w_out"], np.float32),
        "ffn_w_up": np.ascontiguousarray(inp["ffn_w_up"], np.float32), "ffn_w_down": np.ascontiguousarray(inp["ffn_w_down"], np.float32),
        **hc,
    }
    in_maps = [dict(shared, xT=np.ascontiguousarray(x[b].T)) for b in range(B)]
    res = run_bass_kernel_spmd(nc, in_maps, core_ids=list(range(B)))
    out = np.stack([np.asarray(res.results[b]["yT"], np.float32).T for b in range(B)], 0)
    return np.ascontiguousarray(out, dtype=np.float32)
```
